# Optimizing a Trainium2 kernel written in Bass

```python
import jax, jax.numpy as jnp
from jax import lax
import numpy as np

D_MODEL = 1024
BATCH = 16
SEQ = 2048
DEPTH = 1

CHUNK = 64
PREV_CHUNKS = 8
BAND = (PREV_CHUNKS + 1) * CHUNK

HEAD_DIM = 64
RWKV_HEADS = 8
ATT_HEADS = 8
RWKV_WIDTH = RWKV_HEADS * HEAD_DIM
ATT_WIDTH = ATT_HEADS * HEAD_DIM
DECAY_LORA = 64
ICLR_LORA = 64
GATE_LORA = 128
N_BRANCH = 2
REL_CLIP = 128
N_REL = 2 * REL_CLIP + 1

RWKV_COLS = 3 * RWKV_WIDTH + DECAY_LORA + ICLR_LORA + GATE_LORA
ATT_COLS = 3 * ATT_WIDTH
GATE_COLS = N_BRANCH * D_MODEL
IN_COLS = RWKV_COLS + ATT_COLS + GATE_COLS

N_GROUPS = 4
EXPERTS_PER_GROUP = 8
N_EXPERTS = N_GROUPS * EXPERTS_PER_GROUP
TOP_K = 2
D_EXPERT = 512
ROUTE_BLOCK = 128

NORM_EPS = 1e-6
GN_EPS = 64e-5
NEG_INF = -1e30

kernel_name = "hybrid_rwkv7_chunkattn_hiermoe_adaln"


def rms_norm(x, gain):
    xf = x.astype(jnp.float32)
    y = xf * lax.rsqrt(jnp.mean(xf * xf, axis=-1, keepdims=True) + NORM_EPS)
    return (y * gain.astype(jnp.float32)).astype(x.dtype)


def modulate(h, shift, scale):
    return h * (1 + scale[:, None, :]) + shift[:, None, :]


def token_shift(p, mu):
    prev = jnp.pad(p, ((0, 0), (1, 0), (0, 0)))[:, :-1]
    return p + (prev - p) * mu


def rwkv7_branch(p, mu, w0, w_up, a0, a_up, g_up, k_k, k_a, r_k, lnx_g, lnx_b):
    dt = p.dtype
    Bn, S, _ = p.shape
    f32 = jnp.float32
    p = token_shift(p.astype(f32), mu.astype(f32))
    o1, o2, o3 = RWKV_WIDTH, 2 * RWKV_WIDTH, 3 * RWKV_WIDTH
    o4, o5 = o3 + DECAY_LORA, o3 + DECAY_LORA + ICLR_LORA
    r, k, v = p[..., :o1], p[..., o1:o2], p[..., o2:o3]
    wd, ad, gd = p[..., o3:o4], p[..., o4:o5], p[..., o5:]
    w = -jax.nn.softplus(-(w0.astype(f32) + jnp.tanh(wd) @ w_up.astype(f32))) - 0.5
    decay = jnp.exp(-jnp.exp(w))
    a = jax.nn.sigmoid(a0.astype(f32) + ad @ a_up.astype(f32))
    g = jax.nn.sigmoid(gd) @ g_up.astype(f32)

    def heads(t):
        return t.reshape(Bn, S, RWKV_HEADS, HEAD_DIM)

    r, k, v, decay, a = heads(r), heads(k), heads(v), heads(decay), heads(a)
    kk = k * k_k.astype(f32).reshape(RWKV_HEADS, HEAD_DIM)
    kk = kk / jnp.maximum(jnp.sqrt(jnp.sum(kk * kk, axis=-1, keepdims=True)), 1e-12)
    k = k * (1 + (a - 1) * k_a.astype(f32).reshape(RWKV_HEADS, HEAD_DIM))

    def step(state, inp):
        r_t, w_t, k_t, v_t, a_t, b_t = inp
        sa = jnp.einsum('bhvk,bhk->bhv', state, a_t)
        state = (state * w_t[:, :, None, :] + sa[..., None] * b_t[:, :, None, :]
                 + v_t[..., None] * k_t[:, :, None, :])
        return state, jnp.einsum('bhvk,bhk->bhv', state, r_t)

    tm = lambda t: jnp.swapaxes(t, 0, 1)
    state0 = jnp.zeros((Bn, RWKV_HEADS, HEAD_DIM, HEAD_DIM), f32)
    _, y = lax.scan(step, state0, (tm(r), tm(decay), tm(k), tm(v), tm(-kk), tm(kk * a)))
    y = tm(y)
    mean = jnp.mean(y, axis=-1, keepdims=True)
    var = jnp.mean(jnp.square(y - mean), axis=-1, keepdims=True)
    y = ((y - mean) * lax.rsqrt(var + GN_EPS)).reshape(Bn, S, RWKV_WIDTH)
    y = y * lnx_g.astype(f32) + lnx_b.astype(f32)
    bonus = jnp.sum(r * k * r_k.astype(f32), axis=-1, keepdims=True) * v
    y = (y + bonus.reshape(Bn, S, RWKV_WIDTH)) * g
    return y.astype(dt)


def chunk_attention_branch(p, q_g, k_g, rel_bias):
    dt = p.dtype
    Bn, S, _ = p.shape
    f32 = jnp.float32
    pf = p.astype(f32)

    def heads(t):
        return t.reshape(Bn, S, ATT_HEADS, HEAD_DIM).transpose(0, 2, 1, 3)

    q = heads(pf[..., :ATT_WIDTH])
    k = heads(pf[..., ATT_WIDTH:2 * ATT_WIDTH])
    v = heads(pf[..., 2 * ATT_WIDTH:])
    q = rms_norm(q, q_g) * (HEAD_DIM ** -0.5)
    k = rms_norm(k, k_g)
    pad = ((0, 0), (0, 0), (PREV_CHUNKS * CHUNK, 0), (0, 0))
    kp = jnp.pad(k, pad)
    vp = jnp.pad(v, pad)
    q_off = jnp.arange(CHUNK)[:, None]
    k_off = jnp.arange(BAND) - PREV_CHUNKS * CHUNK
    rel_idx = jnp.clip(k_off[None, :] - q_off, -REL_CLIP, REL_CLIP) + REL_CLIP
    bias = rel_bias.astype(f32)[:, rel_idx]

    def one_chunk(ci):
        start = ci * CHUNK
        qc = lax.dynamic_slice_in_dim(q, start, CHUNK, axis=2)
        kb = lax.dynamic_slice_in_dim(kp, start, BAND, axis=2)
        vb = lax.dynamic_slice_in_dim(vp, start, BAND, axis=2)
        s = jnp.einsum('bhqd,bhkd->bhqk', qc, kb) + bias
        valid = (start + k_off) >= 0
        s = jnp.where(valid, s, NEG_INF)
        return jnp.einsum('bhqk,bhkd->bhqd', jax.nn.softmax(s, axis=-1), vb)

    o = lax.map(one_chunk, jnp.arange(S // CHUNK))
    o = o.transpose(1, 0, 3, 2, 4).reshape(Bn, S, ATT_WIDTH)
    return o.astype(dt)


def hier_moe(h, wc, bc, wf, bf, w_gate, w_up, w_down):
    Bn, S, D = h.shape
    f32 = jnp.float32
    n_tok = Bn * S
    xt = h.reshape(n_tok, D)
    xf = xt.astype(f32)
    coarse_p = jax.nn.softmax(xf @ wc.astype(f32) + bc.astype(f32), axis=-1)
    grp = jnp.argmax(coarse_p, axis=-1)
    p_grp = jnp.take_along_axis(coarse_p, grp[:, None], axis=-1)
    fine = (xf @ wf.astype(f32) + bf.astype(f32)).reshape(n_tok, N_GROUPS, EXPERTS_PER_GROUP)
    fine = jnp.take_along_axis(fine, grp[:, None, None], axis=1)[:, 0]
    top_v, top_i = lax.top_k(jax.nn.softmax(fine, axis=-1), TOP_K)
    weights = p_grp * top_v / jnp.sum(top_v, axis=-1, keepdims=True)
    eid = (grp[:, None] * EXPERTS_PER_GROUP + top_i).reshape(-1)

    n_asg = n_tok * TOP_K
    order = jnp.argsort(eid)
    e_sorted = eid[order]
    counts = jnp.bincount(eid, length=N_EXPERTS)
    padded = (counts + ROUTE_BLOCK - 1) // ROUTE_BLOCK * ROUTE_BLOCK
    starts = jnp.cumsum(counts) - counts
    pad_ends = jnp.cumsum(padded)
    pad_starts = pad_ends - padded
    dest = pad_starts[e_sorted] + jnp.arange(n_asg) - starts[e_sorted]
    n_rows = n_asg + N_EXPERTS * ROUTE_BLOCK
    n_blocks = n_rows // ROUTE_BLOCK
    buf = jnp.zeros((n_rows, D), xt.dtype).at[dest].set(xt[order // TOP_K])
    block_e = jnp.minimum(
        jnp.searchsorted(pad_ends, jnp.arange(n_blocks) * ROUTE_BLOCK, side='right'), N_EXPERTS - 1)

    def expert_block(args):
        xb, e = args
        hid = jax.nn.silu(xb @ w_gate[e]) * (xb @ w_up[e])
        return hid @ w_down[e]

    yb = lax.map(expert_block, (buf.reshape(n_blocks, ROUTE_BLOCK, D), block_e)).reshape(n_rows, D)
    y_asg = jnp.zeros((n_asg, D), yb.dtype).at[order].set(yb[dest])
    out = jnp.einsum('nkd,nk->nd', y_asg.reshape(n_tok, TOP_K, D).astype(f32), weights)
    return out.astype(h.dtype).reshape(Bn, S, D)


def setup_inputs(seed: int = 0) -> dict:
    key = jax.random.key(seed)
    ks = iter(jax.random.split(key, 40))
    f32 = jnp.float32
    L = DEPTH

    def nrm(shape, scale):
        return jax.random.normal(next(ks), shape, f32) * scale

    return {
        "x": nrm((BATCH, SEQ, D_MODEL), 1.0),
        "c": nrm((BATCH, D_MODEL), 1.0),
        "w_ada": nrm((L, D_MODEL, 6 * D_MODEL), 0.5 * D_MODEL ** -0.5),
        "b_ada": nrm((L, 6 * D_MODEL), 0.02),
        "norm1_g": 1.0 + nrm((L, D_MODEL), 0.05),
        "w_in": nrm((L, D_MODEL, IN_COLS), D_MODEL ** -0.5),
        "rwkv_mu": jax.random.uniform(next(ks), (L, RWKV_COLS), f32),
        "rwkv_w0": nrm((L, RWKV_WIDTH), 0.5),
        "rwkv_w_up": nrm((L, DECAY_LORA, RWKV_WIDTH), 0.3 * DECAY_LORA ** -0.5),
        "rwkv_a0": nrm((L, RWKV_WIDTH), 0.3),
        "rwkv_a_up": nrm((L, ICLR_LORA, RWKV_WIDTH), 0.3 * ICLR_LORA ** -0.5),
        "rwkv_g_up": nrm((L, GATE_LORA, RWKV_WIDTH), GATE_LORA ** -0.5),
        "rwkv_k_k": 0.85 + nrm((L, RWKV_WIDTH), 0.05),
        "rwkv_k_a": 1.0 + nrm((L, RWKV_WIDTH), 0.05),
        "rwkv_r_k": nrm((L, RWKV_HEADS, HEAD_DIM), 0.1),
        "rwkv_lnx_g": 1.0 + nrm((L, RWKV_WIDTH), 0.05),
        "rwkv_lnx_b": nrm((L, RWKV_WIDTH), 0.02),
        "attn_q_g": 1.0 + nrm((L, HEAD_DIM), 0.05),
        "attn_k_g": 1.0 + nrm((L, HEAD_DIM), 0.05),
        "attn_rel_bias": nrm((L, ATT_HEADS, N_REL), 0.5),
        "w_branch_rwkv": nrm((L, RWKV_WIDTH, D_MODEL), RWKV_WIDTH ** -0.5),
        "w_branch_attn": nrm((L, ATT_WIDTH, D_MODEL), ATT_WIDTH ** -0.5),
        "w_out": nrm((L, D_MODEL, D_MODEL), D_MODEL ** -0.5),
        "norm2_g": 1.0 + nrm((L, D_MODEL), 0.05),
        "router_coarse_w": nrm((L, D_MODEL, N_GROUPS), D_MODEL ** -0.5),
        "router_coarse_b": nrm((L, N_GROUPS), 0.01),
        "router_fine_w": nrm((L, D_MODEL, N_EXPERTS), D_MODEL ** -0.5),
        "router_fine_b": nrm((L, N_EXPERTS), 0.01),
        "expert_w_gate": nrm((L, N_EXPERTS, D_MODEL, D_EXPERT), D_MODEL ** -0.5),
        "expert_w_up": nrm((L, N_EXPERTS, D_MODEL, D_EXPERT), D_MODEL ** -0.5),
        "expert_w_down": nrm((L, N_EXPERTS, D_EXPERT, D_MODEL), D_EXPERT ** -0.5),
    }


def reference(x, c, w_ada, b_ada, norm1_g, w_in, rwkv_mu, rwkv_w0, rwkv_w_up, rwkv_a0,
              rwkv_a_up, rwkv_g_up, rwkv_k_k, rwkv_k_a, rwkv_r_k, rwkv_lnx_g, rwkv_lnx_b,
              attn_q_g, attn_k_g, attn_rel_bias, w_branch_rwkv, w_branch_attn, w_out, norm2_g,
              router_coarse_w, router_coarse_b, router_fine_w, router_fine_b,
              expert_w_gate, expert_w_up, expert_w_down):
    for l in range(DEPTH):
        mod = jax.nn.silu(c) @ w_ada[l] + b_ada[l]
        sh1, sc1, g1, sh2, sc2, g2 = jnp.split(mod, 6, axis=-1)

        h = modulate(rms_norm(x, norm1_g[l]), sh1, sc1)
        p = h @ w_in[l]
        p_rwkv = p[..., :RWKV_COLS]
        p_att = p[..., RWKV_COLS:RWKV_COLS + ATT_COLS]
        p_gate = p[..., RWKV_COLS + ATT_COLS:]
        y_r = rwkv7_branch(p_rwkv, rwkv_mu[l], rwkv_w0[l], rwkv_w_up[l], rwkv_a0[l], rwkv_a_up[l],
                           rwkv_g_up[l], rwkv_k_k[l], rwkv_k_a[l], rwkv_r_k[l],
                           rwkv_lnx_g[l], rwkv_lnx_b[l]) @ w_branch_rwkv[l]
        y_a = chunk_attention_branch(p_att, attn_q_g[l], attn_k_g[l],
                                     attn_rel_bias[l]) @ w_branch_attn[l]
        gate_r = jax.nn.sigmoid(p_gate[..., :D_MODEL])
        gate_a = jax.nn.sigmoid(p_gate[..., D_MODEL:])
        mixed = (gate_r * y_r + gate_a * y_a) @ w_out[l]
        x = x + g1[:, None, :] * mixed

        h2 = modulate(rms_norm(x, norm2_g[l]), sh2, sc2)
        x = x + g2[:, None, :] * hier_moe(h2, router_coarse_w[l], router_coarse_b[l],
                                          router_fine_w[l], router_fine_b[l],
                                          expert_w_gate[l], expert_w_up[l], expert_w_down[l])
    return x
```

```python
import numpy as np
import concourse.bass as bass
import concourse.mybir as mybir
from concourse.bass_utils import run_bass_kernel_spmd

F32 = mybir.dt.float32
BF16 = mybir.dt.bfloat16
AF = mybir.ActivationFunctionType
ALU = mybir.AluOpType
AX = mybir.AxisListType

D = 1024
NCORES = 8
CH = 64
UT = 512
NEXP = 32
DEXP = 512
NORM_EPS = 1e-6
GN_EPS = 64e-5


_BARRIER = {}


class Buf:
    __slots__ = ("w", "r", "excl")

    def __init__(self, excl=False):
        self.excl = excl
        self.w = None
        self.r = dict(_BARRIER)


class Eng:
    def __init__(self, name, h, sem):
        self.name = name
        self.h = h
        self.sem = sem
        self.cnt = 0
        self.seen = {}
        self.prog = []

    def wait(self, tk):
        src, val = tk
        if src is self and val > self.cnt:
            return
        if self.seen.get(src, 0) >= val:
            return
        self.seen[src] = val
        h = self.h
        sem = src.sem
        self.prog.append(lambda: h.wait_ge(sem, val))


class DSem:
    def __init__(self, sem):
        self.sem = sem
        self.val = 0


class FW:
    def __init__(self, nc, nsem_dma=8):
        self.nc = nc
        self._stack = []
        mk = self._sem
        self.pe = Eng("pe", nc.tensor, mk("s_pe"))
        self.dve = Eng("dve", nc.vector, mk("s_dve"))
        self.act = Eng("act", nc.scalar, mk("s_act"))
        self.pool = Eng("pool", nc.gpsimd, mk("s_pool"))
        self.sp = Eng("sp", nc.sync, mk("s_sp"))
        self.dsems = {}
        for e in (self.sp, self.pool):
            self.dsems[e.name] = [DSem(mk(f"d_{e.name}{i}")) for i in range(nsem_dma)]
        self.dptr = {k: 0 for k in self.dsems}

    def _sem(self, name):
        cm = self.nc.semaphore(name)
        s = cm.__enter__()
        self._stack.append(cm)
        return s

    def _deps(self, eng, reads, writes):
        for b in reads:
            if b.w is not None:
                eng.wait(b.w)
            if b.excl:
                for src, val in b.r.items():
                    if src is not eng:
                        eng.wait((src, val))
        for b in writes:
            if b.w is not None:
                eng.wait(b.w)
            for src, val in b.r.items():
                eng.wait((src, val))

    def _mark(self, tk, reads, writes):
        src, val = tk
        for b in reads:
            b.r[src] = val
        for b in writes:
            b.w = tk
            b.r = {}

    def op(self, eng, emit, reads=(), writes=(), inc=True):
        self._deps(eng, reads, writes)
        if inc:
            eng.cnt += 1
            sem = eng.sem
            eng.prog.append(lambda: emit().then_inc(sem, 1))
            tk = (eng, eng.cnt)
        else:
            eng.prog.append(lambda: emit())
            tk = (eng, eng.cnt + 1)
        self._mark(tk, reads, writes)
        return tk

    def dma(self, q, emit, reads=(), writes=()):
        ring = self.dsems[q.name]
        i = self.dptr[q.name]
        self.dptr[q.name] = (i + 1) % len(ring)
        ds = ring[i]
        if ds.val > 0:
            q.wait((ds, ds.val))
        self._deps(q, reads, writes)
        ds.val += 16
        sem = ds.sem
        q.prog.append(lambda: emit().then_inc(sem, 16))
        tk = (ds, ds.val)
        self._mark(tk, reads, writes)
        return tk

    def emit_all(self, final_tickets=()):
        for tk in final_tickets:
            self.sp.wait(tk)
        with self.nc.Block() as block:
            @block.tensor
            def _(e):
                for f in self.pe.prog:
                    f()

            @block.vector
            def _(e):
                for f in self.dve.prog:
                    f()

            @block.scalar
            def _(e):
                for f in self.act.prog:
                    f()

            @block.gpsimd
            def _(e):
                for f in self.pool.prog:
                    f()

            @block.sync
            def _(e):
                for f in self.sp.prog:
                    f()

    def close(self):
        while self._stack:
            self._stack.pop().__exit__(None, None, None)


class K:
    def __init__(self, nc):
        self.nc = nc
        self.fw = FW(nc)
        self.cms = []
        self.banks = []
        self.bptr = 0

    def sb(self, name, shape, dt=F32):
        self._nctr = getattr(self, "_nctr", 0) + 1
        cm = self.nc.sbuf_tensor(f"{name}_{self._nctr}", list(shape), dt)
        t = cm.__enter__()
        self.cms.append(cm)
        return t

    def mark(self):
        return len(self.cms)

    def release(self, m):
        if len(self.cms) > m:
            fw = self.fw
            for e in (fw.pe, fw.dve, fw.act, fw.pool):
                if e.cnt > 0:
                    _BARRIER[e] = e.cnt
            for ring in fw.dsems.values():
                for ds in ring:
                    if ds.val > 0:
                        _BARRIER[ds] = ds.val
        while len(self.cms) > m:
            self.cms.pop().__exit__(None, None, None)

    def init_psum(self):
        for i in range(8):
            cm = self.nc.psum_tensor(f"psb{i}", [128, 512], F32)
            t = cm.__enter__()
            self._pcm = getattr(self, "_pcm", []) + [cm]
            self.banks.append((t, Buf(excl=True)))

    def bank(self, hold=False):
        held = getattr(self, "_held", set())
        while self.bptr in held:
            self.bptr = (self.bptr + 1) % 8
        b = self.banks[self.bptr]
        if hold:
            held.add(self.bptr)
            self._held = held
        self.bptr = (self.bptr + 1) % 8
        return b

    def unhold(self, bank):
        for i, b in enumerate(self.banks):
            if b is bank or b[0] is bank[0]:
                self._held.discard(i)

    def mm(self, out, lhsT, rhs, start=True, stop=True, r=(), w=(), inc=True):
        nc = self.nc
        return self.fw.op(self.fw.pe, lambda: nc.tensor.matmul(out, lhsT=lhsT, rhs=rhs, start=start, stop=stop),
                          reads=r, writes=w, inc=inc)

    def _eng(self, e):
        fw = self.fw
        return {"dve": (fw.dve, self.nc.vector), "act": (fw.act, self.nc.scalar), "pool": (fw.pool, self.nc.gpsimd)}[e]

    def tt(self, e, out, in0, in1, op, r=(), w=()):
        eng, h = self._eng(e)
        return self.fw.op(eng, lambda: h.tensor_tensor(out=out, in0=in0, in1=in1, op=op), reads=r, writes=w)

    def ts(self, e, out, in0, s1, s2, op0, op1=None, r=(), w=()):
        eng, h = self._eng(e)
        if op1 is None:
            return self.fw.op(eng, lambda: h.tensor_scalar(out=out, in0=in0, scalar1=s1, scalar2=None, op0=op0),
                              reads=r, writes=w)
        return self.fw.op(eng, lambda: h.tensor_scalar(out=out, in0=in0, scalar1=s1, scalar2=s2, op0=op0, op1=op1),
                          reads=r, writes=w)

    def stt(self, e, out, in0, scalar, in1, op0, op1, r=(), w=()):
        eng, h = self._eng(e)
        return self.fw.op(eng, lambda: h.scalar_tensor_tensor(out=out, in0=in0, scalar=scalar, in1=in1, op0=op0, op1=op1),
                          reads=r, writes=w)

    def cp(self, e, out, in_, r=(), w=()):
        eng, h = self._eng(e)
        if e == "act":
            return self.fw.op(eng, lambda: h.copy(out=out, in_=in_), reads=r, writes=w)
        return self.fw.op(eng, lambda: h.tensor_copy(out=out, in_=in_), reads=r, writes=w)

    def actf(self, out, in_, func, bias=None, scale=None, accum_out=None, r=(), w=()):
        nc = self.nc
        kw = {}
        if bias is not None:
            kw["bias"] = bias
        if scale is not None:
            kw["scale"] = scale
        if accum_out is not None:
            kw["accum_out"] = accum_out
        return self.fw.op(self.fw.act, lambda: nc.scalar.activation(out=out, in_=in_, func=func, **kw), reads=r, writes=w)

    def red(self, e, out, in_, op, r=(), w=()):
        eng, h = self._eng(e)
        return self.fw.op(eng, lambda: h.tensor_reduce(out=out, in_=in_, axis=AX.X, op=op), reads=r, writes=w)

    def memset(self, e, ap, val, w=()):
        eng, h = self._eng(e)
        return self.fw.op(eng, lambda: h.memset(ap, val), writes=w)

    def recip(self, out, in_, r=(), w=()):
        nc = self.nc
        return self.fw.op(self.fw.dve, lambda: nc.vector.reciprocal(out=out, in_=in_), reads=r, writes=w)

    def rsq(self, out, in_, mul, add, r=(), w=()):
        self.actf(out, in_, AF.Ln, bias=add, scale=mul, r=r, w=w)
        return self.actf(out, out, AF.Exp, scale=-0.5, r=list(r) + list(w), w=w)

    def dma(self, out, in_, r=(), w=(), q="sp"):
        nc = self.nc
        if q == "sp":
            return self.fw.dma(self.fw.sp, lambda: nc.sync.dma_start(out=out, in_=in_), reads=r, writes=w)
        return self.fw.dma(self.fw.pool, lambda: nc.gpsimd.dma_start(out=out, in_=in_), reads=r, writes=w)


def _v3(ap, a):
    return ap.rearrange("p (a b) -> p a b", a=a)


def build(NSEQ=2, S=2048, debug=False, stop_after=None):
    nc = bass.Bass("TRN2", target_bir_lowering=False)
    _BARRIER.clear()
    NTOK = NSEQ * S
    NUB = S // UT
    NTT = S // 128

    def din(name, shape, dt=F32):
        return nc.dram_tensor(name, list(shape), dt, kind="ExternalInput").ap()

    x_d = din("x", [NTOK, D])
    cT_d = din("cT", [128, 8, NSEQ])
    w_ada_d = din("w_ada", [D, 6 * D])
    b_ada_col_d = din("b_ada_col", [128, 48])
    b_ada_row_d = din("b_ada_row", [1, 6 * D])
    ncols_d = din("ncols", [128, 16])
    w_in_d = din("w_in", [D, 5376])
    mu_d = din("mu_cols", [128, 15])
    rwc_d = din("rw_cols", [128, 28])
    wup_d = din("w_up", [64, 512])
    aup_d = din("a_up", [64, 512])
    gup_d = din("g_up", [128, 512])
    qkg_d = din("qkg", [128, 2])
    btab_d = din("btab", [128, 8, 640])
    bmask_d = din("bmask", [128, 640])
    wbr_d = din("w_br", [512, D])
    wba_d = din("w_ba", [512, D])
    wout_d = din("w_out", [D, D])
    wr_d = din("w_router", [D, 36])
    br_d = din("b_router", [1, 36])
    eg_d = din("e_gate", [NEXP, D, DEXP])
    eu_d = din("e_up", [NEXP, D, DEXP])
    ed_d = din("e_down", [NEXP, DEXP, D])
    cst_d = din("consts", [128, 1088])
    out_d = nc.dram_tensor("out", [NTOK, D], F32, kind="ExternalOutput").ap()
    x1_d = nc.dram_tensor("x1s", [NTOK, D], F32, kind="Internal").ap()
    dbg = {}
    if debug:
        dbg["yr"] = nc.dram_tensor("dbg_yr", [512, NTOK], F32, kind="ExternalOutput").ap()
        dbg["oa"] = nc.dram_tensor("dbg_oa", [512, NTOK], F32, kind="ExternalOutput").ap()
        dbg["h"] = nc.dram_tensor("dbg_h", [D, NTOK], F32, kind="ExternalOutput").ap()

    k = K(nc)
    k.init_psum()
    finals = []
    x1_bufs = {}

    cst = k.sb("cst", [128, 1088])
    b_cst = Buf()
    k.dma(cst[:], cst_d, w=[b_cst])
    ident = cst[:, 0:128]
    blockones = cst[:, 128:256]
    maskAT4 = cst[:, 256:768]
    maskA = cst[:, 768:896]
    II = cst[:, 896:960]
    identb = k.sb("identb", [128, 128], BF16)
    blockonesb = k.sb("blockonesb", [128, 128], BF16)
    b_cb = Buf()
    k.cp("dve", identb[:], ident, r=[b_cst], w=[b_cb])
    k.cp("dve", blockonesb[:], blockones, r=[b_cst], w=[b_cb])

    ncols = k.sb("ncols", [128, 16])
    mu = k.sb("mu", [128, 15])
    omu = k.sb("omu", [128, 15])
    rwc = k.sb("rwc", [128, 28])
    qkg = k.sb("qkg", [128, 2])
    badac = k.sb("badac", [128, 48])
    b_par = Buf()
    k.dma(ncols[:], ncols_d, w=[b_par])
    k.dma(mu[:], mu_d, w=[b_par])
    k.dma(rwc[:], rwc_d, w=[b_par])
    k.dma(qkg[:], qkg_d, w=[b_par])
    k.dma(badac[:], b_ada_col_d, w=[b_par])
    b_par2 = Buf()
    k.ts("dve", omu[:], mu[:], -1.0, 1.0, ALU.mult, ALU.add, r=[b_par], w=[b_par2])
    k.ts("dve", qkg[:, 0:1], qkg[:, 0:1], 0.125, None, ALU.mult, r=[b_par], w=[b_par])
    W0, A0, KK_, KA, RK, LG, LB = [rwc[:, 4 * i:4 * i + 4] for i in range(7)]

    cT = k.sb("cT", [128, 8, NSEQ])
    scb = k.sb("scb", [128, 8, NSEQ], BF16)
    screp = k.sb("screp", [128, 8, NSEQ, 128], BF16)
    modc = k.sb("modc", [128, 4, 8, NSEQ])
    AB = k.sb("AB", [128, 4, 8, NSEQ])
    b_sc = Buf()
    b_modc = Buf()
    b_AB = Buf()
    k.dma(cT[:], cT_d, w=[b_sc])
    k.actf(scb[:], cT[:], AF.Silu, r=[b_sc], w=[b_sc])
    for b in range(NSEQ):
        k.cp("dve", screp[:, :, b, :], scb[:, :, b:b + 1].to_broadcast([128, 8, 128]), r=[b_sc], w=[b_sc])
    m_mod = k.mark()
    wa = [k.sb(f"wa{i}", [128, 8, 1024], BF16) for i in range(2)]
    b_wa = [Buf(), Buf()]
    for vi, blk in enumerate((0, 1, 3, 4)):
        wt, bw = wa[vi % 2], b_wa[vi % 2]
        k.dma(wt[:], w_ada_d[:, blk * 1024:(blk + 1) * 1024].rearrange("(kt p) c -> p kt c", p=128), w=[bw], q="pool")
        ps, bp = k.bank()
        for ct in range(8):
            for kt in range(8):
                k.mm(ps[:, ct * NSEQ:(ct + 1) * NSEQ], wt[:, kt, ct * 128:(ct + 1) * 128], scb[:, kt, :],
                     start=(kt == 0), stop=(kt == 7), r=[bw, b_sc], w=[bp], inc=(ct == 7 and kt == 7))
        k.tt("dve", modc[:, vi, :, :], _v3(ps[:, 0:8 * NSEQ], 8),
             badac[:, blk * 8:(blk + 1) * 8].unsqueeze(2).to_broadcast([128, 8, NSEQ]), ALU.add,
             r=[bp, b_par], w=[b_modc])
    for half in range(2):
        gcol = ncols[:, half * 8:(half + 1) * 8].unsqueeze(2).to_broadcast([128, 8, NSEQ])
        k.stt("dve", AB[:, 2 * half, :, :], modc[:, 2 * half + 1, :, :], 1.0, gcol, ALU.add, ALU.mult,
              r=[b_modc, b_par], w=[b_AB])
        k.cp("dve", AB[:, 2 * half + 1, :, :], modc[:, 2 * half, :, :], r=[b_modc], w=[b_AB])
    k.release(m_mod)

    def norm_to_fm(xt, bx, hsel, seq, outs, tmp):
        ss, sq, xn, rstd, b_t = tmp
        k.actf(sq[:], xt, AF.Square, accum_out=ss[:, 0:1], r=[bx], w=[b_t])
        k.rsq(rstd[:, 0:1], ss[:, 0:1], 1.0 / D, NORM_EPS, r=[b_t], w=[b_t])
        k.actf(xn[:], xt, AF.Copy, scale=rstd[:, 0:1], r=[bx, b_t], w=[b_t])
        for half in range(2):
            ps, bp = k.bank()
            for j in range(4):
                kt = half * 4 + j
                k.mm(ps[:, j * 128:(j + 1) * 128], xn[:, kt * 128:(kt + 1) * 128], ident if xn.dtype == F32 else identb[:],
                     r=[b_t, b_cst, b_cb], w=[bp], inc=(j == 3))
            for j in range(4):
                kt = half * 4 + j
                for oi, (dst, bd) in enumerate(outs):
                    eng = "act" if half == 0 else "dve"
                    if eng == "act":
                        k.actf(dst[:, kt, :], ps[:, j * 128:(j + 1) * 128], AF.Identity,
                               bias=AB[:, 2 * hsel + 1, kt, seq:seq + 1], scale=AB[:, 2 * hsel, kt, seq:seq + 1],
                               r=[bp, b_AB], w=[bd])
                    else:
                        k.ts("dve", dst[:, kt, :], ps[:, j * 128:(j + 1) * 128],
                             AB[:, 2 * hsel, kt, seq:seq + 1], AB[:, 2 * hsel + 1, kt, seq:seq + 1], ALU.mult, ALU.add,
                             r=[bp, b_AB], w=[bd])

    m_seq = k.mark()
    for seq in range(NSEQ):
        k.release(m_seq)
        tok0 = seq * S
        hT = k.sb("hT", [128, 8, S], BF16)
        b_hT = Buf()
        yrT = k.sb("yrT", [128, 4, S], BF16)
        b_yrT = Buf()

        mA = k.mark()
        xts = [k.sb(f"xt{i}", [128, D]) for i in range(2)]
        bxs = [Buf(), Buf()]
        tmpA = []
        for i in range(2):
            tmpA.append((k.sb(f"ssA{i}", [128, 1]), k.sb(f"sqA{i}", [128, D], BF16), k.sb(f"xnA{i}", [128, D], BF16),
                         k.sb(f"rstdA{i}", [128, 1]), Buf()))
        for tt in range(NTT):
            xt, bx = xts[tt % 2], bxs[tt % 2]
            k.dma(xt[:], x_d[tok0 + tt * 128: tok0 + (tt + 1) * 128, :], w=[bx])
            k.memset("pool", tmpA[tt % 2][0][:], 0.0, w=[tmpA[tt % 2][4]])
            norm_to_fm(xt[:], bx, 0, seq, [(hT[:, :, tt * 128:(tt + 1) * 128], b_hT)], tmpA[tt % 2])
        k.release(mA)
        if debug:
            mD = k.mark()
            hf = k.sb("hf_dbg", [128, 8, S])
            bdbg = Buf()
            k.cp("dve", hf[:], hT[:], r=[b_hT], w=[bdbg])
            finals.append(k.dma(dbg["h"][:, tok0:tok0 + S].rearrange("(kt p) t -> p kt t", p=128), hf[:], r=[bdbg], w=[Buf()]))
            k.release(mD)

        mR = k.mark()
        if stop_after != "A":
            rwkv_branch(k, nc, locals())
        k.release(mR)
        if debug:
            mD = k.mark()
            yf = k.sb("yf_dbg", [128, 4, S])
            bdbg = Buf()
            k.cp("dve", yf[:], yrT[:], r=[b_yrT], w=[bdbg])
            finals.append(k.dma(dbg["yr"][:, tok0:tok0 + S].rearrange("(kt p) t -> p kt t", p=128), yf[:], r=[bdbg], w=[Buf()]))
            k.release(mD)
        if stop_after in ("A", "R"):
            continue

        oT = k.sb("oT", [128, 4, S], BF16)
        b_oT = Buf()
        mAt = k.mark()
        attention_branch(k, nc, locals())
        k.release(mAt)
        if debug:
            mD = k.mark()
            of = k.sb("of_dbg", [128, 4, S])
            bdbg = Buf()
            k.cp("dve", of[:], oT[:], r=[b_oT], w=[bdbg])
            finals.append(k.dma(dbg["oa"][:, tok0:tok0 + S].rearrange("(kt p) t -> p kt t", p=128), of[:], r=[bdbg], w=[Buf()]))
            k.release(mD)
        if stop_after == "D":
            continue

        mM = k.mark()
        merge_phase(k, nc, locals())
        k.release(mM)
    k.release(m_seq)

    if stop_after is None:
        for seq in range(NSEQ):
            mE = k.mark()
            moe_phase(k, nc, locals())
            k.release(mE)

    k.fw.emit_all(finals)
    k.release(0)
    for cm in reversed(k._pcm):
        cm.__exit__(None, None, None)
    k.fw.close()
    return nc


import os
RW_STOP = int(os.environ.get("RW_STOP", "99"))
RW_NCH = int(os.environ.get("RW_NCH", "99"))
RW_SUB = int(os.environ.get("RW_SUB", "99"))


def rwkv_branch(k, nc, L):
    S, seq = L["S"], L["seq"]
    hT, b_hT, yrT, b_yrT = L["hT"], L["b_hT"], L["yrT"], L["b_yrT"]
    w_in_d, mu, omu, b_par, b_par2 = L["w_in_d"], L["mu"], L["omu"], L["b_par"], L["b_par2"]
    W0, A0, KK_, KA, RK, LG, LB = L["W0"], L["A0"], L["KK_"], L["KA"], L["RK"], L["LG"], L["LB"]
    ident, blockones, maskAT4, maskA, II, b_cst = L["ident"], L["blockones"], L["maskAT4"], L["maskA"], L["II"], L["b_cst"]
    NUB = S // UT
    NCK = UT // CH
    GC = 4
    NG = NCK // GC

    wrw = k.sb("wrw", [128, 8, 1792], BF16)
    b_wrw = Buf()
    for i in range(7):
        k.dma(wrw[:, :, i * 256:(i + 1) * 256],
              w_in_d[:, i * 256:(i + 1) * 256].rearrange("(kt p) c -> p kt c", p=128), w=[b_wrw], q="pool")
    wup = k.sb("wup", [64, 512], BF16)
    aup = k.sb("aup", [64, 512], BF16)
    gup = k.sb("gup", [128, 512], BF16)
    b_lw = Buf()
    k.dma(wup[:], L["wup_d"], w=[b_lw], q="pool")
    k.dma(aup[:], L["aup_d"], w=[b_lw], q="pool")
    k.dma(gup[:], L["gup_d"], w=[b_lw], q="pool")

    carry = k.sb("carry", [128, 15])
    b_carry = Buf()
    k.memset("pool", carry[:], 0.0, w=[b_carry])
    Tst = k.sb("Tst", [128, 4, 64])
    b_Tst = [Buf() for _ in range(4)]
    k.memset("pool", Tst[:], 0.0, w=b_Tst)

    twd = k.sb("twd", [64, UT], BF16)
    tad = k.sb("tad", [64, UT], BF16)
    sgd = k.sb("sgd", [128, UT], BF16)
    b_sh = Buf()
    praw = [k.sb(f"praw{i}", [128, UT + 1]) for i in range(2)]
    b_praw = [Buf(), Buf()]
    pctr = [0]
    T = [k.sb(f"T{i}", [128, UT]) for i in range(6)]
    bT = [Buf() for _ in range(6)]
    AR = k.sb("AR", [128, NCK, 256])
    BK = k.sb("BK", [128, NCK, 256])
    VX = k.sb("VX", [128, NCK, 128])
    b_AR, b_BK, b_VX = Buf(), Buf(), Buf()
    k.memset("pool", AR[:], 0.0, w=[b_AR])
    k.memset("pool", BK[:], 0.0, w=[b_BK])
    k.memset("pool", VX[:], 0.0, w=[b_VX])
    WL = k.sb("WL", [128, NCK])
    b_WL = Buf()
    AT = k.sb("AT", [128, GC, 512])
    b_ATg = Buf()
    PP = [k.sb(f"PP{i}", [128, GC, 256]) for i in range(2)]
    b_PP = [Buf(), Buf()]
    MT = k.sb("MT", [128, GC, 128])
    b_MT = Buf()
    TOK = k.sb("TOK", [128, GC, 448])
    b_TOK = Buf()
    AXp = k.sb("AXp", [128, GC, 128])
    b_AXp = Buf()
    k.memset("pool", AXp[:], 0.0, w=[b_AXp])
    Vp = k.sb("Vp", [128, GC, 64])
    b_Vp = Buf()
    G0T = k.sb("G0T", [128, GC, 128])
    b_G0T = Buf()
    Hh = k.sb("Hh", [128, GC, 64])
    b_H = Buf()
    RpT = k.sb("RpT", [128, GC, 128])
    b_RpT = Buf()
    TS = k.sb("TS", [128, GC + 1, 64])
    b_TS = Buf()
    ysb = k.sb("ysb", [128, GC, 64])
    ysq = k.sb("ysq", [128, GC, 64])
    yst = k.sb("yst", [128, 4, GC])
    b_y = Buf()
    YX = k.sb("YX", [128, GC, 128])
    b_YX = Buf()
    k.memset("pool", YX[:], 0.0, w=[b_YX])
    yfm = k.sb("yfm", [128, UT])
    b_yfm = Buf()

    def project(ct_cols, M, mu_col, ub, dst, bdst, carry_idx):
        pr, bpr = praw[pctr[0] % 2], b_praw[pctr[0] % 2]
        pctr[0] += 1
        ps, bp = k.bank()
        for kt in range(8):
            k.mm(ps[0:M, :], wrw[:, kt, ct_cols:ct_cols + M], hT[:, kt, ub * UT:(ub + 1) * UT],
                 start=(kt == 0), stop=(kt == 7), r=[b_wrw, b_hT], w=[bp], inc=(kt == 7))
        k.cp("pool", pr[0:M, 0:1], carry[0:M, carry_idx:carry_idx + 1], r=[b_carry], w=[bpr])
        k.cp("act", pr[0:M, 1:UT + 1], ps[0:M, :], r=[bp], w=[bpr])
        k.cp("pool", carry[0:M, carry_idx:carry_idx + 1], pr[0:M, UT:UT + 1], r=[bpr], w=[b_carry])
        k.ts("dve", dst, pr[0:M, 1:UT + 1], omu[0:M, mu_col:mu_col + 1], None, ALU.mult, r=[bpr, b_par2], w=[bdst])
        k.stt("dve", dst, pr[0:M, 0:UT], mu[0:M, mu_col:mu_col + 1], dst, ALU.mult, ALU.add, r=[bpr, b_par], w=[bdst])

    if RW_STOP <= 1:
        return
    for ub in range(NUB):
        tsl = slice(ub * UT, (ub + 1) * UT)
        project(1536, 64, 12, ub, T[0][0:64, :], bT[0], 12)
        k.actf(twd[:], T[0][0:64, :], AF.Tanh, r=[bT[0]], w=[b_sh])
        project(1600, 64, 13, ub, T[1][0:64, :], bT[1], 13)
        k.cp("dve", tad[:], T[1][0:64, :], r=[bT[1]], w=[b_sh])
        project(1664, 128, 14, ub, T[2][:], bT[2], 14)
        k.actf(sgd[:], T[2][:], AF.Sigmoid, r=[bT[2]], w=[b_sh])

        if RW_STOP <= 2:
            return
        for hp in range(4):
            c4 = slice(hp, hp + 1)
            hcol = slice(hp * 128, (hp + 1) * 128)
            ps, bp = k.bank()
            k.mm(ps[:, :], wup[:, hcol], twd[:], r=[b_lw, b_sh], w=[bp])
            k.actf(T[0][:], ps[:, :], AF.Sigmoid, bias=W0[:, c4], r=[bp, b_par], w=[bT[0]])
            k.ts("dve", T[0][:], T[0][:], -0.6065306597126334, None, ALU.mult, r=[bT[0]], w=[bT[0]])
            src, dst, bs, bd = T[0], T[1], bT[0], bT[1]
            sh = 1
            while sh < CH:
                s3, d3 = _v3(src[:], NCK), _v3(dst[:], NCK)
                k.cp("pool", d3[:, :, 0:sh], s3[:, :, 0:sh], r=[bs], w=[bd])
                k.tt("dve", d3[:, :, sh:CH], s3[:, :, sh:CH], s3[:, :, 0:CH - sh], ALU.add, r=[bs], w=[bd])
                src, dst, bs, bd = dst, src, bd, bs
                sh *= 2
            k.actf(T[1][:], T[0][:], AF.Exp, r=[bT[0]], w=[bT[1]])
            k.actf(T[0][:], T[0][:], AF.Exp, scale=-1.0, r=[bT[0]], w=[bT[0]])
            Wt, Winv, bW, bWinv = T[1], T[0], bT[1], bT[0]
            k.cp("pool", WL[:], _v3(Wt[:], NCK)[:, :, CH - 1], r=[bW], w=[b_WL])
            ps, bp = k.bank()
            k.mm(ps[:, :], aup[:, hcol], tad[:], r=[b_lw, b_sh], w=[bp])
            k.actf(T[2][:], ps[:, :], AF.Sigmoid, bias=A0[:, c4], r=[bp, b_par], w=[bT[2]])
            project(512 + hp * 128, 128, 4 + hp, ub, T[3][:], bT[3], 4 + hp)
            k.ts("dve", T[4][:], T[3][:], KK_[:, c4], None, ALU.mult, r=[bT[3], b_par], w=[bT[4]])
            k.tt("pool", T[5][:], T[4][:], T[4][:], ALU.mult, r=[bT[4]], w=[bT[5]])
            ps, bp = k.bank()
            k.mm(ps[:, :], blockones, T[5][:], r=[b_cst, bT[5]], w=[bp])
            k.rsq(T[5][:], ps[:, :], 1.0, 1e-12, r=[bp], w=[bT[5]])
            k.tt("dve", T[4][:], T[4][:], T[5][:], ALU.mult, r=[bT[4], bT[5]], w=[bT[4]])
            k.ts("dve", T[5][:], T[2][:], -1.0, KA[:, c4], ALU.add, ALU.mult, r=[bT[2], b_par], w=[bT[5]])
            k.stt("dve", T[3][:], T[5][:], 1.0, T[3][:], ALU.add, ALU.mult, r=[bT[5], bT[3]], w=[bT[3]])
            k.tt("pool", T[5][:], T[4][:], T[2][:], ALU.mult, r=[bT[4], bT[2]], w=[bT[5]])
            for h2 in range(2):
                pr_ = slice(h2 * 64, (h2 + 1) * 64)
                k.tt("dve", BK[pr_, :, h2 * 64:(h2 + 1) * 64], _v3(T[5][pr_, :], NCK), _v3(Winv[pr_, :], NCK), ALU.mult,
                     r=[bT[5], bWinv], w=[b_BK])
                k.tt("dve", BK[pr_, :, 128 + h2 * 64:128 + (h2 + 1) * 64], _v3(T[3][pr_, :], NCK), _v3(Winv[pr_, :], NCK),
                     ALU.mult, r=[bT[3], bWinv], w=[b_BK])
                kk3, W3 = _v3(T[4][pr_, :], NCK), _v3(Wt[pr_, :], NCK)
                k.stt("dve", AR[pr_, :, h2 * 64 + 1:(h2 + 1) * 64], kk3[:, :, 1:CH], -1.0, W3[:, :, 0:CH - 1],
                      ALU.mult, ALU.mult, r=[bT[4], bW], w=[b_AR])
                k.ts("dve", AR[pr_, :, h2 * 64:h2 * 64 + 1], kk3[:, :, 0:1], -1.0, None, ALU.mult, r=[bT[4]], w=[b_AR])
            project(hp * 128, 128, hp, ub, T[2][:], bT[2], hp)
            for h2 in range(2):
                pr_ = slice(h2 * 64, (h2 + 1) * 64)
                k.tt("dve", AR[pr_, :, 128 + h2 * 64:128 + (h2 + 1) * 64], _v3(T[2][pr_, :], NCK), _v3(Wt[pr_, :], NCK),
                     ALU.mult, r=[bT[2], bW], w=[b_AR])
            k.stt("dve", T[4][:], T[2][:], RK[:, c4], T[3][:], ALU.mult, ALU.mult, r=[bT[2], bT[3], b_par], w=[bT[4]])
            psb, bpb = k.bank()
            k.mm(psb[:, :], blockones, T[4][:], r=[b_cst, bT[4]], w=[bpb])
            project(1024 + hp * 128, 128, 8 + hp, ub, T[3][:], bT[3], 8 + hp)
            k.tt("dve", T[4][:], psb[:, :], T[3][:], ALU.mult, r=[bpb, bT[3]], w=[bT[4]])
            for h2 in range(2):
                pr_ = slice(h2 * 64, (h2 + 1) * 64)
                k.cp("pool", VX[pr_, :, h2 * 64:(h2 + 1) * 64], _v3(T[3][pr_, :], NCK), r=[bT[3]], w=[b_VX])
            ps, bp = k.bank()
            k.mm(ps[:, :], gup[:, hcol], sgd[:], r=[b_lw, b_sh], w=[bp])
            k.cp("act", T[5][:], ps[:, :], r=[bp], w=[bT[5]])
            bonus, gT, b_bonus, b_g = T[4], T[5], bT[4], bT[5]

            if RW_STOP <= 3:
                return
            for g in range(NG):
                c0 = g * GC
                for ci in range(GC):
                    c = c0 + ci
                    if ci >= RW_NCH:
                        return
                    psA, bpA = k.bank()
                    k.mm(psA[:, 0:256], BK[:, c, 0:128], AR[:, c, :], r=[b_BK, b_AR], w=[bpA], inc=False)
                    k.mm(psA[:, 256:512], BK[:, c, 128:256], AR[:, c, :], r=[b_BK, b_AR], w=[bpA])
                    k.tt("dve", AT[:, ci, :], psA[:, :], maskAT4, ALU.mult, r=[bpA, b_cst], w=[b_ATg])
                    if RW_SUB <= 0:
                        continue
                    psB, bpB = k.bank()
                    k.mm(psB[:, 0:128], AR[:, c, 0:128], BK[:, c, 0:128], r=[b_AR, b_BK], w=[bpB], inc=False)
                    k.mm(psB[:, 128:192], AR[:, c, 0:128], II, r=[b_AR, b_cst], w=[bpB], inc=False)
                    k.mm(psB[:, 192:256], VX[:, c, :], II, r=[b_VX, b_cst], w=[bpB], inc=False)
                    k.mm(psB[:, 256:384], BK[:, c, 0:128], ident, r=[b_BK, b_cst], w=[bpB], inc=False)
                    k.mm(psB[:, 384:512], BK[:, c, 128:256], ident, r=[b_BK, b_cst], w=[bpB])
                    k.tt("dve", PP[0][:, ci, 0:128], psB[:, 0:128], maskA, ALU.mult, r=[bpB, b_cst], w=[b_PP[0]])
                    k.cp("dve", TOK[:, ci, 0:64], psB[:, 128:192], r=[bpB], w=[b_TOK])
                    k.cp("dve", TOK[:, ci, 128:448], psB[:, 192:512], r=[bpB], w=[b_TOK])
                if RW_SUB <= 1:
                    return
                k.cp("pool", PP[0][:, :, 128:256], AT[:, :, 0:128], r=[b_ATg], w=[b_PP[0]])
                k.tt("pool", MT[:], AT[:, :, 0:128], ident.unsqueeze(1).to_broadcast([128, GC, 128]), ALU.add,
                     r=[b_ATg, b_cst], w=[b_MT])
                if RW_STOP <= 4:
                    return
                cur = 0
                for lev in range(1, 6):
                    Pc, bPc = PP[cur], b_PP[cur]
                    Pn, bPn = PP[1 - cur], b_PP[1 - cur]
                    for half in range(2):
                        ps, bp = k.bank()
                        for cj in range(2):
                            ci = half * 2 + cj
                            k.mm(ps[:, cj * 256:cj * 256 + 128], Pc[:, ci, 128:256], Pc[:, ci, 0:128], r=[bPc], w=[bp], inc=False)
                            k.mm(ps[:, cj * 256 + 128:cj * 256 + 256], Pc[:, ci, 0:128], Pc[:, ci, 128:256], r=[bPc], w=[bp],
                                 inc=(cj == 1))
                        k.cp("act", Pn[:, half * 2:half * 2 + 2, :], _v3(ps[:, :], 2), r=[bp], w=[bPn])
                    ps, bp = k.bank()
                    for ci in range(GC):
                        k.mm(ps[:, ci * 128:(ci + 1) * 128], Pn[:, ci, 0:128], MT[:, ci, :], r=[bPn, b_MT], w=[bp], inc=(ci == GC - 1))
                    k.tt("dve", MT[:], MT[:], _v3(ps[:, :], GC), ALU.add, r=[bp, b_MT], w=[b_MT])
                    cur = 1 - cur
                if RW_STOP <= 5:
                    return
                ps, bp = k.bank()
                for ci in range(GC):
                    k.mm(ps[:, ci * 64:(ci + 1) * 64], AT[:, ci, 256:384], TOK[:, ci, 128:192], r=[b_ATg, b_TOK], w=[bp], inc=(ci == GC - 1))
                k.cp("act", TOK[:, :, 64:128], _v3(ps[:, 0:GC * 64], GC), r=[bp], w=[b_TOK])
                ps, bp = k.bank()
                for ci in range(GC):
                    k.mm(ps[:, ci * 128:(ci + 1) * 128], MT[:, ci, :], TOK[:, ci, 0:128], r=[b_MT, b_TOK], w=[bp], inc=(ci == GC - 1))
                p3 = _v3(ps[:, :], GC)
                k.cp("act", AXp[0:64, :, 0:64], p3[0:64, :, 0:64], r=[bp], w=[b_AXp])
                k.cp("act", AXp[64:128, :, 64:128], p3[64:128, :, 0:64], r=[bp], w=[b_AXp])
                k.cp("act", Vp[:], p3[:, :, 64:128], r=[bp], w=[b_Vp])
                psG, bpG = k.bank()
                psR, bpR = k.bank()
                psH, bpH = k.bank()
                for ci in range(GC):
                    last = (ci == GC - 1)
                    k.mm(psG[:, ci * 128:(ci + 1) * 128], AXp[:, ci, :], TOK[:, ci, 192:320], r=[b_AXp, b_TOK], w=[bpG], inc=last)
                    k.mm(psR[:, ci * 128:(ci + 1) * 128], AXp[:, ci, :], AT[:, ci, 128:256], r=[b_AXp, b_ATg], w=[bpR], inc=last)
                    k.mm(psH[:, ci * 64:(ci + 1) * 64], TOK[:, ci, 192:320], Vp[:, ci, :], start=True, stop=False,
                         r=[b_TOK, b_Vp], w=[bpH], inc=False)
                    k.mm(psH[:, ci * 64:(ci + 1) * 64], TOK[:, ci, 320:448], TOK[:, ci, 128:192], start=False, stop=True,
                         r=[b_TOK], w=[bpH], inc=last)
                k.tt("dve", G0T[:], _v3(psG[:, :], GC), ident.unsqueeze(1).to_broadcast([128, GC, 128]), ALU.add,
                     r=[bpG, b_cst], w=[b_G0T])
                k.tt("dve", RpT[:], _v3(psR[:, :], GC), AR[:, c0:c0 + GC, 128:256], ALU.add, r=[bpR, b_AR], w=[b_RpT])
                k.tt("dve", Hh[:], _v3(psH[:, 0:GC * 64], GC), WL[:, c0:c0 + GC].unsqueeze(2).to_broadcast([128, GC, 64]),
                     ALU.mult, r=[bpH, b_WL], w=[b_H])
                if RW_STOP <= 6:
                    return
                k.cp("pool", TS[:, 0, :], Tst[:, hp, :], r=[b_Tst[hp]], w=[b_TS])
                for ci in range(GC):
                    c = c0 + ci
                    ps, bp = k.bank()
                    k.mm(ps[:, 0:64], G0T[:, ci, :], TS[:, ci, :], r=[b_G0T, b_TS], w=[bp])
                    k.stt("dve", TS[:, ci + 1, :], ps[:, 0:64], WL[:, c:c + 1], Hh[:, ci, :], ALU.mult, ALU.add,
                          r=[bp, b_WL, b_H], w=[b_TS])
                k.cp("pool", Tst[:, hp, :], TS[:, GC, :], r=[b_TS], w=[b_Tst[hp]])
                if RW_STOP <= 7:
                    return
                ps, bp = k.bank()
                for ci in range(GC):
                    o = ps[:, ci * 64:(ci + 1) * 64]
                    k.mm(o, AT[:, ci, 128:256], Vp[:, ci, :], start=True, stop=False, r=[b_ATg, b_Vp], w=[bp], inc=False)
                    k.mm(o, AT[:, ci, 384:512], TOK[:, ci, 128:192], start=False, stop=False, r=[b_ATg, b_TOK], w=[bp], inc=False)
                    k.mm(o, RpT[:, ci, :], TS[:, ci, :], start=False, stop=True, r=[b_RpT, b_TS], w=[bp], inc=(ci == GC - 1))
                k.cp("act", ysb[:], _v3(ps[:, 0:GC * 64], GC), r=[bp], w=[b_y])
                k.red("dve", yst[:, 0, :], ysb[:], ALU.add, r=[b_y], w=[b_y])
                k.tt("pool", ysq[:], ysb[:], ysb[:], ALU.mult, r=[b_y], w=[b_y])
                k.red("dve", yst[:, 1, :], ysq[:], ALU.add, r=[b_y], w=[b_y])
                k.ts("dve", yst[:, 2, :], yst[:, 0, :], 1.0 / 64, None, ALU.mult, r=[b_y], w=[b_y])
                k.tt("dve", yst[:, 3, :], yst[:, 2, :], yst[:, 2, :], ALU.mult, r=[b_y], w=[b_y])
                k.stt("dve", yst[:, 3, :], yst[:, 1, :], 1.0 / 64, yst[:, 3, :], ALU.mult, ALU.subtract, r=[b_y], w=[b_y])
                k.rsq(yst[:, 3, :], yst[:, 3, :], 1.0, GN_EPS, r=[b_y], w=[b_y])
                k.tt("dve", ysb[:], ysb[:], yst[:, 2, :].unsqueeze(2).to_broadcast([128, GC, 64]), ALU.subtract, r=[b_y], w=[b_y])
                for h2 in range(2):
                    pr_ = slice(h2 * 64, (h2 + 1) * 64)
                    k.tt("dve", YX[pr_, :, h2 * 64:(h2 + 1) * 64], ysb[pr_, :, :],
                         yst[pr_, 3, :].unsqueeze(2).to_broadcast([64, GC, 64]), ALU.mult, r=[b_y], w=[b_YX])
                ps, bp = k.bank()
                for ci in range(GC):
                    k.mm(ps[:, ci * 64:(ci + 1) * 64], YX[:, ci, :], II, r=[b_YX, b_cst], w=[bp], inc=(ci == GC - 1))
                k.actf(yfm[:, c0 * CH:(c0 + GC) * CH], ps[:, 0:GC * 64], AF.Identity, bias=LB[:, c4], scale=LG[:, c4],
                       r=[bp, b_par], w=[b_yfm])
            k.tt("dve", yfm[:], yfm[:], bonus[:], ALU.add, r=[b_yfm, b_bonus], w=[b_yfm])
            k.tt("dve", yrT[:, hp, tsl], yfm[:], gT[:], ALU.mult, r=[b_yfm, b_g], w=[b_yrT])


def attention_branch(k, nc, L):
    S, seq = L["S"], L["seq"]
    hT, b_hT, oT, b_oT = L["hT"], L["b_hT"], L["oT"], L["b_oT"]
    w_in_d, qkg, b_par = L["w_in_d"], L["qkg"], L["b_par"]
    identb, blockonesb, b_cb = L["identb"], L["blockonesb"], L["b_cb"]
    NTT = S // 128
    NB = S // 512
    wat = k.sb("wat", [128, 8, 1536], BF16)
    b_wat = Buf()
    for i in range(6):
        k.dma(wat[:, :, i * 256:(i + 1) * 256],
              w_in_d[:, 1792 + i * 256:1792 + (i + 1) * 256].rearrange("(kt p) c -> p kt c", p=128), w=[b_wat], q="pool")
    EB = k.sb("EB", [128, 8, 640], BF16)
    b_EB = Buf()
    bm = k.sb("bm", [128, 640])
    b_bm = Buf()
    k.dma(bm[:], L["bmask_d"], w=[b_bm])
    bst = [k.sb(f"bst{i}", [128, 640]) for i in range(2)]
    b_bst = [Buf(), Buf()]
    for h in range(8):
        t, bt = bst[h % 2], b_bst[h % 2]
        k.dma(t[:], L["btab_d"][:, h, :], w=[bt])
        k.tt("pool", t[:], t[:], bm[:], ALU.add, r=[bt, b_bm], w=[bt])
        k.actf(EB[:, h, :], t[:], AF.Exp, r=[bt], w=[b_EB])

    qT = k.sb("qT", [128, 4, S], BF16)
    kT = k.sb("kT", [128, 4, S], BF16)
    b_qT, b_kT = Buf(), Buf()
    Va = k.sb("Va", [128, NTT, 8, 65], BF16)
    b_Va = Buf()
    k.memset("pool", Va[:], 1.0, w=[b_Va])
    sqb = [k.sb(f"sqb{i}", [128, 512], BF16) for i in range(2)]
    raw = [k.sb(f"qraw{i}", [128, 512]) for i in range(2)]
    rs = [k.sb(f"qrs{i}", [128, 512]) for i in range(2)]
    b_n = [Buf(), Buf()]
    it = 0
    for which in range(2):
        dstT, bdst = (qT, b_qT) if which == 0 else (kT, b_kT)
        for ct in range(4):
            col0 = which * 512 + ct * 128
            for blk in range(NB):
                i = it % 2
                it += 1
                ps, bp = k.bank()
                for kt in range(8):
                    k.mm(ps[:, :], wat[:, kt, col0:col0 + 128], hT[:, kt, blk * 512:(blk + 1) * 512],
                         start=(kt == 0), stop=(kt == 7), r=[b_wat, b_hT], w=[bp], inc=(kt == 7))
                k.cp("dve", raw[i][:], ps[:, :], r=[bp], w=[b_n[i]])
                k.actf(sqb[i][:], raw[i][:], AF.Square, r=[b_n[i]], w=[b_n[i]])
                ps2, bp2 = k.bank()
                k.mm(ps2[:, :], blockonesb[:], sqb[i][:], r=[b_cb, b_n[i]], w=[bp2])
                k.rsq(rs[i][:], ps2[:, :], 1.0 / 64, NORM_EPS, r=[bp2], w=[b_n[i]])
                k.stt("dve", dstT[:, ct, blk * 512:(blk + 1) * 512], raw[i][:], qkg[:, which:which + 1], rs[i][:],
                      ALU.mult, ALU.mult, r=[b_n[i], b_par], w=[bdst])
    for tt in range(NTT):
        ps, bp = k.bank()
        for kt in range(8):
            k.mm(ps[:, :], hT[:, kt, tt * 128:(tt + 1) * 128], wat[:, kt, 1024:1536],
                 start=(kt == 0), stop=(kt == 7), r=[b_wat, b_hT], w=[bp], inc=(kt == 7))
        k.cp("act", Va[:, tt, :, 0:64], _v3(ps[:, :], 8), r=[bp], w=[b_Va])

    PT = [k.sb(f"PT{i}", [128, 640], BF16) for i in range(3)]
    b_PT = [Buf() for _ in range(3)]
    otok = [k.sb(f"otok{i}", [128, 512], BF16) for i in range(2)]
    b_otok = [Buf(), Buf()]
    rec = [k.sb(f"rec{i}", [128, 4]) for i in range(2)]
    b_rec = [Buf(), Buf()]
    pi = 0
    for p in range(NTT):
        j0 = max(0, 4 - p)
        ot, bot = otok[p % 2], b_otok[p % 2]
        for hh in range(2):
            pso_b = k.bank(hold=True)
            pso, bpo = pso_b
            for h4 in range(4):
                h = hh * 4 + h4
                ct, pb = h // 2, (h % 2) * 64
                Pt, bPt = PT[pi % 3], b_PT[pi % 3]
                pi += 1
                ps1, bp1 = k.bank()
                ps2, bp2 = k.bank()
                for j in range(j0, 5):
                    tt = p - 4 + j
                    dst = ps1[:, j * 128:(j + 1) * 128] if j < 4 else ps2[:, 0:128]
                    k.mm(dst, kT[pb:pb + 64, ct, tt * 128:(tt + 1) * 128], qT[pb:pb + 64, ct, p * 128:(p + 1) * 128],
                         r=[b_kT, b_qT], w=[bp1 if j < 4 else bp2], inc=(j == 3 or j == 4))
                if j0 < 4:
                    k.actf(Pt[:, j0 * 128:512], ps1[:, j0 * 128:512], AF.Exp, r=[bp1], w=[bPt])
                k.actf(Pt[:, 512:640], ps2[:, 0:128], AF.Exp, r=[bp2], w=[bPt])
                k.tt("pool", Pt[:, j0 * 128:640], Pt[:, j0 * 128:640], EB[:, h, j0 * 128:640],
                     ALU.mult, r=[bPt, b_EB], w=[bPt])
                for j in range(j0, 5):
                    tt = p - 4 + j
                    k.mm(pso[:, h4 * 65:(h4 + 1) * 65], Pt[:, j * 128:(j + 1) * 128], Va[:, tt, h, :],
                         start=(j == j0), stop=(j == 4), r=[bPt, b_Va], w=[bpo], inc=(j == 4))
            po3 = pso[:, 0:260].rearrange("p (a b) -> p a b", a=4)
            rc, brc = rec[hh], b_rec[hh]
            k.recip(rc[:], po3[:, :, 64], r=[bpo], w=[brc])
            k.tt("dve", _v3(ot[:, hh * 256:(hh + 1) * 256], 4), po3[:, :, 0:64],
                 rc[:].unsqueeze(2).to_broadcast([128, 4, 64]), ALU.mult, r=[bpo, brc], w=[bot])
            k.unhold(pso_b)
        ps, bp = k.bank()
        for ct in range(4):
            k.mm(ps[:, ct * 128:(ct + 1) * 128], ot[:, ct * 128:(ct + 1) * 128], identb[:], r=[bot, b_cb], w=[bp], inc=(ct == 3))
        k.cp("act", oT[:, :, p * 128:(p + 1) * 128], _v3(ps[:, :], 4), r=[bp], w=[b_oT])


def merge_phase(k, nc, L):
    S, seq, tok0 = L["S"], L["seq"], L["tok0"]
    hT, b_hT, oT, b_oT, yrT, b_yrT = L["hT"], L["b_hT"], L["oT"], L["b_oT"], L["yrT"], L["b_yrT"]
    w_in_d, w_ada_d, screp, b_sc = L["w_in_d"], L["w_ada_d"], L["screp"], L["b_sc"]
    x_d, x1_d = L["x_d"], L["x1_d"]
    NB = S // 512
    wg = k.sb("wg", [128, 8, 2048], BF16)
    b_wg = Buf()
    for i in range(8):
        k.dma(wg[:, :, i * 256:(i + 1) * 256],
              w_in_d[:, 3328 + i * 256:3328 + (i + 1) * 256].rearrange("(kt p) c -> p kt c", p=128), w=[b_wg], q="pool")
    wbr = k.sb("wbr", [128, 4, 1024], BF16)
    wba = k.sb("wba", [128, 4, 1024], BF16)
    wo = k.sb("wo", [128, 8, 1024], BF16)
    b_w = Buf()
    k.dma(wbr[:], L["wbr_d"].rearrange("(kt p) c -> p kt c", p=128), w=[b_w], q="pool")
    k.dma(wba[:], L["wba_d"].rearrange("(kt p) c -> p kt c", p=128), w=[b_w], q="pool")
    for i in range(2):
        k.dma(wo[:, :, i * 512:(i + 1) * 512], L["wout_d"][:, i * 512:(i + 1) * 512].rearrange("(kt p) c -> p kt c", p=128),
              w=[b_w], q="pool")
    g1row = k.sb("g1row", [128, 1024])
    b_g1 = Buf()
    mG = k.mark()
    wag = k.sb("wag", [128, 8, 1024], BF16)
    b_wag = Buf()
    brow = k.sb("brow", [128, 1024])
    b_brow = Buf()
    k.dma(wag[:], w_ada_d[:, 2048:3072].rearrange("(kt p) c -> p kt c", p=128), w=[b_wag], q="pool")
    k.dma(brow[:], L["b_ada_row_d"][0:1, 2048:3072].partition_broadcast(128), w=[b_brow])
    for half in range(2):
        ps, bp = k.bank()
        for kt in range(8):
            k.mm(ps[:, :], screp[:, kt, seq, :], wag[:, kt, half * 512:(half + 1) * 512], start=(kt == 0), stop=(kt == 7),
                 r=[b_sc, b_wag], w=[bp], inc=(kt == 7))
        k.tt("dve", g1row[:, half * 512:(half + 1) * 512], ps[:, :], brow[:, half * 512:(half + 1) * 512], ALU.add,
             r=[bp, b_brow], w=[b_g1])
    k.release(mG)

    sg = k.sb("sg", [128, 16, 512], BF16)
    b_sg = Buf()
    mixT = k.sb("mixT", [128, 8, 512], BF16)
    b_mix = Buf()
    t1 = [k.sb(f"mt1_{i}", [128, 512]) for i in range(2)]
    t2 = [k.sb(f"mt2_{i}", [128, 512]) for i in range(2)]
    b_t = [Buf(), Buf()]
    xt = [k.sb(f"mxt{i}", [128, 1024]) for i in range(2)]
    b_xt = [Buf(), Buf()]
    x1t = [k.sb(f"mx1t{i}", [128, 1024]) for i in range(2)]
    b_x1t = [Buf(), Buf()]
    ti = 0
    for blk in range(NB):
        bsl = slice(blk * 512, (blk + 1) * 512)
        for ct in range(16):
            ps, bp = k.bank()
            for kt in range(8):
                k.mm(ps[:, :], wg[:, kt, ct * 128:(ct + 1) * 128], hT[:, kt, bsl], start=(kt == 0), stop=(kt == 7),
                     r=[b_wg, b_hT], w=[bp], inc=(kt == 7))
            k.actf(sg[:, ct, :], ps[:, :], AF.Sigmoid, r=[bp], w=[b_sg])
        for dt_ in range(8):
            i = dt_ % 2
            psr, bpr = k.bank()
            for kt in range(4):
                k.mm(psr[:, :], wbr[:, kt, dt_ * 128:(dt_ + 1) * 128], yrT[:, kt, bsl], start=(kt == 0), stop=(kt == 3),
                     r=[b_w, b_yrT], w=[bpr], inc=(kt == 3))
            psa, bpa = k.bank()
            for kt in range(4):
                k.mm(psa[:, :], wba[:, kt, dt_ * 128:(dt_ + 1) * 128], oT[:, kt, bsl], start=(kt == 0), stop=(kt == 3),
                     r=[b_w, b_oT], w=[bpa], inc=(kt == 3))
            k.tt("dve", t1[i][:], psr[:, :], sg[:, dt_, :], ALU.mult, r=[bpr, b_sg], w=[b_t[i]])
            k.tt("dve", t2[i][:], psa[:, :], sg[:, 8 + dt_, :], ALU.mult, r=[bpa, b_sg], w=[b_t[i]])
            k.tt("pool", mixT[:, dt_, :], t1[i][:], t2[i][:], ALU.add, r=[b_t[i]], w=[b_mix])
        for tq in range(4):
            i = ti % 2
            ti += 1
            r0 = tok0 + blk * 512 + tq * 128
            k.dma(xt[i][:], x_d[r0:r0 + 128, :], w=[b_xt[i]])
            for half in range(2):
                ps, bp = k.bank()
                for kt in range(8):
                    k.mm(ps[:, :], mixT[:, kt, tq * 128:(tq + 1) * 128], wo[:, kt, half * 512:(half + 1) * 512],
                         start=(kt == 0), stop=(kt == 7), r=[b_mix, b_w], w=[bp], inc=(kt == 7))
                hs = slice(half * 512, (half + 1) * 512)
                k.tt("dve", x1t[i][:, hs], ps[:, :], g1row[:, hs], ALU.mult, r=[bp, b_g1], w=[b_x1t[i]])
                k.tt("pool", x1t[i][:, hs], x1t[i][:, hs], xt[i][:, hs], ALU.add, r=[b_x1t[i], b_xt[i]], w=[b_x1t[i]])
            L["x1_bufs"][r0 // 128] = Buf()
            k.dma(x1_d[r0:r0 + 128, :], x1t[i][:], r=[b_x1t[i]], w=[L["x1_bufs"][r0 // 128]])


def moe_phase(k, nc, L):
    S, seq = L["S"], L["seq"]
    tok0 = seq * S
    NTT = S // 128
    NB = S // 512
    x1_d, out_d = L["x1_d"], L["out_d"]
    ident, b_cst, AB, b_AB, screp, b_sc = L["ident"], L["b_cst"], L["AB"], L["b_AB"], L["screp"], L["b_sc"]
    finals = L["finals"]
    g2row = k.sb("g2row", [128, 1024])
    b_g2 = Buf()
    mG = k.mark()
    wag = k.sb("wag2", [128, 8, 1024], BF16)
    b_wag = Buf()
    brow = k.sb("brow2", [128, 1024])
    b_brow = Buf()
    k.dma(wag[:], L["w_ada_d"][:, 5120:6144].rearrange("(kt p) c -> p kt c", p=128), w=[b_wag], q="pool")
    k.dma(brow[:], L["b_ada_row_d"][0:1, 5120:6144].partition_broadcast(128), w=[b_brow])
    for half in range(2):
        ps, bp = k.bank()
        for kt in range(8):
            k.mm(ps[:, :], screp[:, kt, seq, :], wag[:, kt, half * 512:(half + 1) * 512], start=(kt == 0), stop=(kt == 7),
                 r=[b_sc, b_wag], w=[bp], inc=(kt == 7))
        k.tt("dve", g2row[:, half * 512:(half + 1) * 512], ps[:, :], brow[:, half * 512:(half + 1) * 512], ALU.add,
             r=[bp, b_brow], w=[b_g2])
    k.release(mG)

    wr = k.sb("wr", [128, 8, 36])
    brt = k.sb("brt", [128, 36])
    b_wr = Buf()
    k.dma(wr[:], L["wr_d"].rearrange("(kt p) c -> p kt c", p=128), w=[b_wr])
    k.dma(brt[:], L["br_d"][0:1, :].partition_broadcast(128), w=[b_wr])

    h2b = k.sb("h2b", [128, 8, S], BF16)
    b_h2b = Buf()
    Wt = k.sb("Wt", [128, NTT, 32])
    b_Wt = [Buf() for _ in range(NTT)]
    acc = k.sb("acc", [128, NTT, 1024])
    b_acc = [Buf() for _ in range(NTT)]

    mP = k.mark()
    xts = [k.sb(f"ext{i}", [128, D]) for i in range(2)]
    bxs = [Buf(), Buf()]
    tmpA = []
    for i in range(2):
        tmpA.append((k.sb(f"ess{i}", [128, 1]), k.sb(f"esq{i}", [128, D]), k.sb(f"exn{i}", [128, D]),
                     k.sb(f"erstd{i}", [128, 1]), Buf()))
    h2f = [k.sb(f"h2f{i}", [128, 8, 128]) for i in range(2)]
    b_h2f = [Buf(), Buf()]
    lg = k.sb("lg", [128, 36])
    rt = k.sb("rt", [128, 64])
    fs = k.sb("fs", [128, 4, 8])
    b_rt = Buf()
    for tt in range(NTT):
        i = tt % 2
        r0 = tok0 + tt * 128
        k.dma(xts[i][:], x1_d[r0:r0 + 128, :], r=[L["x1_bufs"][r0 // 128]], w=[bxs[i]])
        k.memset("pool", tmpA[i][0][:], 0.0, w=[tmpA[i][4]])
        L["norm_to_fm"](xts[i][:], bxs[i], 1, seq, [(h2f[i][:], b_h2f[i])], tmpA[i])
        k.cp("pool", h2b[:, :, tt * 128:(tt + 1) * 128], h2f[i][:], r=[b_h2f[i]], w=[b_h2b])
        ps, bp = k.bank()
        for kt in range(8):
            k.mm(ps[:, 0:36], h2f[i][:, kt, :], wr[:, kt, :], start=(kt == 0), stop=(kt == 7), r=[b_h2f[i], b_wr], w=[bp],
                 inc=(kt == 7))
        k.tt("dve", lg[:], ps[:, 0:36], brt[:], ALU.add, r=[bp, b_wr], w=[b_rt])
        cm, ce, csum, oh = rt[:, 0:1], rt[:, 4:8], rt[:, 1:2], rt[:, 8:12]
        k.red("dve", cm, lg[:, 0:4], ALU.max, r=[b_rt], w=[b_rt])
        k.ts("dve", oh, lg[:, 0:4], cm, None, ALU.is_equal, r=[b_rt], w=[b_rt])
        k.ts("dve", ce, lg[:, 0:4], cm, None, ALU.subtract, r=[b_rt], w=[b_rt])
        k.actf(ce, ce, AF.Exp, r=[b_rt], w=[b_rt])
        k.red("dve", csum, ce, ALU.add, r=[b_rt], w=[b_rt])
        pg = rt[:, 2:3]
        k.recip(pg, csum, r=[b_rt], w=[b_rt])
        k.tt("dve", fs[:], _v3(lg[:, 4:36], 4), oh.unsqueeze(2).to_broadcast([128, 4, 8]), ALU.mult, r=[b_rt], w=[b_rt])
        fsel = rt[:, 16:24]
        k.tt("dve", rt[:, 24:32], fs[:, 0, :], fs[:, 1, :], ALU.add, r=[b_rt], w=[b_rt])
        k.tt("dve", rt[:, 32:40], fs[:, 2, :], fs[:, 3, :], ALU.add, r=[b_rt], w=[b_rt])
        k.tt("dve", fsel, rt[:, 24:32], rt[:, 32:40], ALU.add, r=[b_rt], w=[b_rt])
        m1, m2 = rt[:, 40:41], rt[:, 41:42]
        oh1, oh2 = rt[:, 24:32], rt[:, 32:40]
        k.red("dve", m1, fsel, ALU.max, r=[b_rt], w=[b_rt])
        k.ts("dve", oh1, fsel, m1, None, ALU.is_equal, r=[b_rt], w=[b_rt])
        msk = rt[:, 48:56]
        k.stt("dve", msk, oh1, -1e30, fsel, ALU.mult, ALU.add, r=[b_rt], w=[b_rt])
        k.red("dve", m2, msk, ALU.max, r=[b_rt], w=[b_rt])
        k.ts("dve", oh2, msk, m2, None, ALU.is_equal, r=[b_rt], w=[b_rt])
        e2, den, w1, w2 = rt[:, 42:43], rt[:, 43:44], rt[:, 44:45], rt[:, 45:46]
        k.tt("dve", e2, m2, m1, ALU.subtract, r=[b_rt], w=[b_rt])
        k.actf(e2, e2, AF.Exp, r=[b_rt], w=[b_rt])
        k.ts("dve", den, e2, 1.0, None, ALU.add, r=[b_rt], w=[b_rt])
        k.recip(den, den, r=[b_rt], w=[b_rt])
        k.tt("dve", w1, den, pg, ALU.mult, r=[b_rt], w=[b_rt])
        k.tt("dve", w2, w1, e2, ALU.mult, r=[b_rt], w=[b_rt])
        w8 = rt[:, 56:64]
        k.ts("dve", w8, oh1, w1, None, ALU.mult, r=[b_rt], w=[b_rt])
        k.stt("dve", w8, oh2, w2, w8, ALU.mult, ALU.add, r=[b_rt], w=[b_rt])
        k.tt("dve", _v3(Wt[:, tt, :], 4), oh.unsqueeze(2).to_broadcast([128, 4, 8]),
             w8.unsqueeze(1).to_broadcast([128, 4, 8]), ALU.mult, r=[b_rt], w=[b_Wt[tt]])
    k.release(mP)

    weg = [k.sb(f"weg{i}", [128, 8, 512], BF16) for i in range(2)]
    weu = [k.sb(f"weu{i}", [128, 8, 512], BF16) for i in range(2)]
    wed = [k.sb(f"wed{i}", [128, 4, 1024], BF16) for i in range(2)]
    b_we = [Buf(), Buf()]
    hid = k.sb("hid", [128, 4, 512], BF16)
    b_hid = Buf()
    sgt = [k.sb(f"sgt{i}", [128, 512]) for i in range(2)]
    b_sgt = [Buf(), Buf()]
    for e in range(NEXP):
        i = e % 2
        k.dma(weg[i][:], L["eg_d"][e].rearrange("(kt p) c -> p kt c", p=128), w=[b_we[i]], q="pool")
        k.dma(weu[i][:], L["eu_d"][e].rearrange("(kt p) c -> p kt c", p=128), w=[b_we[i]], q="pool")
        k.dma(wed[i][:], L["ed_d"][e].rearrange("(kt p) c -> p kt c", p=128), w=[b_we[i]], q="pool")
        for blk in range(NB):
            bsl = slice(blk * 512, (blk + 1) * 512)
            for ht in range(4):
                j = ht % 2
                psg, bpg = k.bank()
                for kt in range(8):
                    k.mm(psg[:, :], weg[i][:, kt, ht * 128:(ht + 1) * 128], h2b[:, kt, bsl], start=(kt == 0), stop=(kt == 7),
                         r=[b_we[i], b_h2b], w=[bpg], inc=(kt == 7))
                psu, bpu = k.bank()
                for kt in range(8):
                    k.mm(psu[:, :], weu[i][:, kt, ht * 128:(ht + 1) * 128], h2b[:, kt, bsl], start=(kt == 0), stop=(kt == 7),
                         r=[b_we[i], b_h2b], w=[bpu], inc=(kt == 7))
                k.actf(sgt[j][:], psg[:, :], AF.Silu, r=[bpg], w=[b_sgt[j]])
                k.tt("dve", hid[:, ht, :], psu[:, :], sgt[j][:], ALU.mult, r=[bpu, b_sgt[j]], w=[b_hid])
            for tq in range(4):
                tt = blk * 4 + tq
                for half in range(2):
                    ps, bp = k.bank()
                    for kt in range(4):
                        k.mm(ps[:, :], hid[:, kt, tq * 128:(tq + 1) * 128], wed[i][:, kt, half * 512:(half + 1) * 512],
                             start=(kt == 0), stop=(kt == 3), r=[b_hid, b_we[i]], w=[bp], inc=(kt == 3))
                    hs = slice(half * 512, (half + 1) * 512)
                    if e == 0:
                        k.ts("dve", acc[:, tt, hs], ps[:, :], Wt[:, tt, e:e + 1], None, ALU.mult, r=[bp, b_Wt[tt]], w=[b_acc[tt]])
                    else:
                        k.stt("dve", acc[:, tt, hs], ps[:, :], Wt[:, tt, e:e + 1], acc[:, tt, hs], ALU.mult, ALU.add,
                              r=[bp, b_Wt[tt]], w=[b_acc[tt]])
    xo = [k.sb(f"xo{i}", [128, 1024]) for i in range(2)]
    b_xo = [Buf(), Buf()]
    for tt in range(NTT):
        i = tt % 2
        r0 = tok0 + tt * 128
        k.dma(xo[i][:], x1_d[r0:r0 + 128, :], r=[L["x1_bufs"][r0 // 128]], w=[b_xo[i]])
        k.tt("pool", acc[:, tt, :], acc[:, tt, :], g2row[:], ALU.mult, r=[b_acc[tt], b_g2], w=[b_acc[tt]])
        k.tt("dve", xo[i][:], xo[i][:], acc[:, tt, :], ALU.add, r=[b_acc[tt], b_xo[i]], w=[b_xo[i]])
        finals.append(k.dma(out_d[r0:r0 + 128, :], xo[i][:], r=[b_xo[i]], w=[Buf()]))


def _consts():
    c = np.zeros((128, 1088), np.float32)
    c[:, 0:128] = np.eye(128)
    bo = np.zeros((128, 128), np.float32)
    bo[:64, :64] = 1
    bo[64:, 64:] = 1
    c[:, 128:256] = bo
    j = np.arange(128)[:, None]
    s = np.arange(128)[None, :]
    same = (j // 64) == (s // 64)
    strict = same & ((j % 64) < (s % 64))
    incl = same & ((j % 64) <= (s % 64))
    c[:, 256:384] = strict
    c[:, 384:512] = incl
    c[:, 512:640] = strict
    c[:, 640:768] = incl
    c[:, 768:896] = same & ((s % 64) < (j % 64))
    ii = np.zeros((128, 64), np.float32)
    ii[np.arange(128), np.arange(128) % 64] = 1
    c[:, 896:960] = ii
    c[:, 960:1088] = 1.0
    return c


def _attn_tables(rel_bias):
    kl = np.arange(128)[:, None, None]
    j = np.arange(5)[None, :, None]
    q = np.arange(128)[None, None, :]
    off = (j - 4) * 128 + kl - q
    idx = np.clip(off, -128, 128) + 128
    tab = rel_bias[:, idx]
    tab = np.ascontiguousarray(tab.transpose(1, 0, 2, 3)).reshape(128, 8, 640)
    kc = 2 * j + kl // 64
    qc = q // 64
    vis = (kc >= qc) & (kc <= qc + 8)
    mask = np.where(vis, 0.0, -30000.0).astype(np.float32).reshape(128, 640)
    return tab.astype(np.float32), mask


def _col(v, n):
    return np.ascontiguousarray(np.asarray(v, np.float32).reshape(n, 128).T)


def prep_inputs(inputs, NSEQ=2, S=2048, ncores=NCORES):
    f = lambda n: np.asarray(inputs[n], np.float32)
    shared = {}
    shared["w_ada"] = np.ascontiguousarray(f("w_ada")[0])
    shared["b_ada_col"] = _col(f("b_ada")[0], 48)
    shared["b_ada_row"] = np.ascontiguousarray(f("b_ada")[0].reshape(1, -1))
    shared["ncols"] = np.concatenate([_col(f("norm1_g")[0], 8), _col(f("norm2_g")[0], 8)], axis=1)
    shared["w_in"] = np.ascontiguousarray(f("w_in")[0])
    mu = f("rwkv_mu")[0]
    mc = np.zeros((128, 15), np.float32)
    mc[:, 0:12] = _col(mu[0:1536], 12)
    mc[0:64, 12] = mu[1536:1600]
    mc[0:64, 13] = mu[1600:1664]
    mc[:, 14] = mu[1664:1792]
    shared["mu_cols"] = mc
    rw = [f("rwkv_w0")[0], f("rwkv_a0")[0], f("rwkv_k_k")[0], f("rwkv_k_a")[0], f("rwkv_r_k")[0].reshape(-1),
          f("rwkv_lnx_g")[0], f("rwkv_lnx_b")[0]]
    shared["rw_cols"] = np.concatenate([_col(v, 4) for v in rw], axis=1)
    shared["w_up"] = np.ascontiguousarray(f("rwkv_w_up")[0])
    shared["a_up"] = np.ascontiguousarray(f("rwkv_a_up")[0])
    shared["g_up"] = np.ascontiguousarray(f("rwkv_g_up")[0])
    shared["qkg"] = np.stack([np.tile(f("attn_q_g")[0], 2), np.tile(f("attn_k_g")[0], 2)], axis=1).astype(np.float32)
    tab, mask = _attn_tables(f("attn_rel_bias")[0])
    shared["btab"] = tab
    shared["bmask"] = mask
    shared["w_br"] = np.ascontiguousarray(f("w_branch_rwkv")[0])
    shared["w_ba"] = np.ascontiguousarray(f("w_branch_attn")[0])
    shared["w_out"] = np.ascontiguousarray(f("w_out")[0])
    shared["w_router"] = np.ascontiguousarray(np.concatenate([f("router_coarse_w")[0], f("router_fine_w")[0]], axis=1))
    shared["b_router"] = np.concatenate([f("router_coarse_b")[0], f("router_fine_b")[0]]).reshape(1, 36).astype(np.float32)
    shared["e_gate"] = np.ascontiguousarray(f("expert_w_gate")[0])
    shared["e_up"] = np.ascontiguousarray(f("expert_w_up")[0])
    shared["e_down"] = np.ascontiguousarray(f("expert_w_down")[0])
    shared["consts"] = _consts()
    x = f("x")
    c = f("c")
    maps = []
    for i in range(ncores):
        m = dict(shared)
        m["x"] = np.ascontiguousarray(x[i * NSEQ:(i + 1) * NSEQ, :S].reshape(NSEQ * S, D))
        cT = c[i * NSEQ:(i + 1) * NSEQ].T
        m["cT"] = np.ascontiguousarray(cT.reshape(8, 128, NSEQ).transpose(1, 0, 2))
        maps.append(m)
    return maps


_NC_CACHE = {}


def kernel(**inputs):
    NSEQ, S = 2, 2048
    if "nc" not in _NC_CACHE:
        _NC_CACHE["nc"] = build(NSEQ, S)
    nc = _NC_CACHE["nc"]
    maps = prep_inputs(inputs, NSEQ, S)
    res = run_bass_kernel_spmd(nc, maps, core_ids=list(range(NCORES)))
    out = np.concatenate([np.asarray(r["out"], np.float32).reshape(NSEQ, S, D) for r in res.results], axis=0)
    return out
```

```python
import numpy as np
import concourse.bass as bass
import concourse.mybir as mybir
from concourse.bass_utils import run_bass_kernel_spmd

F32 = mybir.dt.float32
BF16 = mybir.dt.bfloat16
AF = mybir.ActivationFunctionType
ALU = mybir.AluOpType
AX = mybir.AxisListType

D = 1024
NCORES = 8
CH = 64
UT = 512
NEXP = 32
DEXP = 512
NORM_EPS = 1e-6
GN_EPS = 64e-5


_BARRIER = {}


class Buf:
    __slots__ = ("w", "r", "excl")

    def __init__(self, excl=False):
        self.excl = excl
        self.w = None
        self.r = dict(_BARRIER)


class SemBox:
    def __init__(self, owner, sem):
        self.owner = owner
        self.sem = sem
        self.cnt = 0


EPOCH = 2000


class Eng:
    def __init__(self, name, h, mk):
        self.name = name
        self.h = h
        self.mk = mk
        self.nbox = 0
        self.cur = None
        self.total = 0
        self.seen = {}
        self.prog = []
        self.new_box()

    def new_box(self):
        self.nbox += 1
        self.prev = self.cur
        self.cur = SemBox(self, self.mk(f"s_{self.name}{self.nbox}"))

    def next_ticket(self):
        if self.cur.cnt >= EPOCH:
            self.new_box()
        return (self.cur, self.cur.cnt + 1)

    def wait(self, tk):
        src, val = tk
        if isinstance(src, SemBox) and src.owner is self and src is self.cur and val > src.cnt:
            return
        if self.seen.get(src, 0) >= val:
            return
        self.seen[src] = val
        h = self.h
        sem = src.sem
        self.prog.append(lambda: h.wait_ge(sem, val))


class DSem:
    def __init__(self, sem):
        self.sem = sem
        self.val = 0


class FW:
    def __init__(self, nc, nsem_dma=8):
        self.nc = nc
        self._stack = []
        mk = self._sem
        self.pe = Eng("pe", nc.tensor, mk)
        self.dve = Eng("dve", nc.vector, mk)
        self.act = Eng("act", nc.scalar, mk)
        self.pool = Eng("pool", nc.gpsimd, mk)
        self.sp = Eng("sp", nc.sync, mk)
        self.dsems = {}
        for e in (self.sp, self.pool):
            self.dsems[e.name] = [DSem(mk(f"d_{e.name}{i}")) for i in range(nsem_dma)]
        self.dptr = {k: 0 for k in self.dsems}

    def _sem(self, name):
        cm = self.nc.semaphore(name)
        s = cm.__enter__()
        self._stack.append(cm)
        return s

    def _deps(self, eng, reads, writes):
        for b in reads:
            if b.w is not None:
                eng.wait(b.w)
            if b.excl:
                for src, val in b.r.items():
                    if src is not eng:
                        eng.wait((src, val))
        for b in writes:
            if b.w is not None:
                eng.wait(b.w)
            for src, val in b.r.items():
                eng.wait((src, val))

    def _mark(self, tk, reads, writes):
        src, val = tk
        for b in reads:
            b.r[src] = val
        for b in writes:
            b.w = tk
            b.r = {}

    def op(self, eng, emit, reads=(), writes=(), inc=True):
        self._deps(eng, reads, writes)
        if inc:
            tk = eng.next_ticket()
            box = tk[0]
            box.cnt += 1
            eng.total += 1
            sem = box.sem
            eng.prog.append(lambda: emit().then_inc(sem, 1))
        else:
            eng.prog.append(lambda: emit())
            tk = eng.next_ticket()
        self._mark(tk, reads, writes)
        return tk

    def dma(self, q, emit, reads=(), writes=()):
        ring = self.dsems[q.name]
        i = self.dptr[q.name]
        self.dptr[q.name] = (i + 1) % len(ring)
        ds = ring[i]
        if ds.val > 0:
            q.wait((ds, ds.val))
        self._deps(q, reads, writes)
        ds.val += 16
        sem = ds.sem
        q.prog.append(lambda: emit().then_inc(sem, 16))
        tk = (ds, ds.val)
        self._mark(tk, reads, writes)
        return tk

    def emit_all(self, final_tickets=()):
        for tk in final_tickets:
            self.sp.wait(tk)
        with self.nc.Block() as block:
            @block.tensor
            def _(e):
                for f in self.pe.prog:
                    f()

            @block.vector
            def _(e):
                for f in self.dve.prog:
                    f()

            @block.scalar
            def _(e):
                for f in self.act.prog:
                    f()

            @block.gpsimd
            def _(e):
                for f in self.pool.prog:
                    f()

            @block.sync
            def _(e):
                for f in self.sp.prog:
                    f()

    def close(self):
        while self._stack:
            self._stack.pop().__exit__(None, None, None)


class K:
    def __init__(self, nc):
        self.nc = nc
        self.fw = FW(nc)
        self.cms = []
        self.banks = []
        self.bptr = 0

    def sb(self, name, shape, dt=F32):
        self._nctr = getattr(self, "_nctr", 0) + 1
        cm = self.nc.sbuf_tensor(f"{name}_{self._nctr}", list(shape), dt)
        t = cm.__enter__()
        self.cms.append(cm)
        return t

    def mark(self):
        return len(self.cms)

    def release(self, m):
        if len(self.cms) > m:
            fw = self.fw
            for e in (fw.pe, fw.dve, fw.act, fw.pool):
                if e.cur.cnt > 0:
                    _BARRIER[e.cur] = e.cur.cnt
                elif e.prev is not None and e.prev.cnt > 0:
                    _BARRIER[e.prev] = e.prev.cnt
            for ring in fw.dsems.values():
                for ds in ring:
                    if ds.val > 0:
                        _BARRIER[ds] = ds.val
        while len(self.cms) > m:
            self.cms.pop().__exit__(None, None, None)

    def init_psum(self):
        for i in range(8):
            cm = self.nc.psum_tensor(f"psb{i}", [128, 512], F32)
            t = cm.__enter__()
            self._pcm = getattr(self, "_pcm", []) + [cm]
            self.banks.append((t, Buf(excl=True)))

    def bank(self, hold=False):
        held = getattr(self, "_held", set())
        while self.bptr in held:
            self.bptr = (self.bptr + 1) % 8
        b = self.banks[self.bptr]
        if hold:
            held.add(self.bptr)
            self._held = held
        self.bptr = (self.bptr + 1) % 8
        return b

    def unhold(self, bank):
        for i, b in enumerate(self.banks):
            if b is bank or b[0] is bank[0]:
                self._held.discard(i)

    def mm(self, out, lhsT, rhs, start=True, stop=True, r=(), w=(), inc=True):
        nc = self.nc
        return self.fw.op(self.fw.pe, lambda: nc.tensor.matmul(out, lhsT=lhsT, rhs=rhs, start=start, stop=stop),
                          reads=r, writes=w, inc=inc)

    def _eng(self, e):
        fw = self.fw
        return {"dve": (fw.dve, self.nc.vector), "act": (fw.act, self.nc.scalar), "pool": (fw.pool, self.nc.gpsimd)}[e]

    def tt(self, e, out, in0, in1, op, r=(), w=()):
        eng, h = self._eng(e)
        return self.fw.op(eng, lambda: h.tensor_tensor(out=out, in0=in0, in1=in1, op=op), reads=r, writes=w)

    def ts(self, e, out, in0, s1, s2, op0, op1=None, r=(), w=()):
        eng, h = self._eng(e)
        if op1 is None:
            return self.fw.op(eng, lambda: h.tensor_scalar(out=out, in0=in0, scalar1=s1, scalar2=None, op0=op0),
                              reads=r, writes=w)
        return self.fw.op(eng, lambda: h.tensor_scalar(out=out, in0=in0, scalar1=s1, scalar2=s2, op0=op0, op1=op1),
                          reads=r, writes=w)

    def stt(self, e, out, in0, scalar, in1, op0, op1, r=(), w=()):
        eng, h = self._eng(e)
        return self.fw.op(eng, lambda: h.scalar_tensor_tensor(out=out, in0=in0, scalar=scalar, in1=in1, op0=op0, op1=op1),
                          reads=r, writes=w)

    def cp(self, e, out, in_, r=(), w=()):
        eng, h = self._eng(e)
        if e == "act":
            return self.fw.op(eng, lambda: h.copy(out=out, in_=in_), reads=r, writes=w)
        return self.fw.op(eng, lambda: h.tensor_copy(out=out, in_=in_), reads=r, writes=w)

    def actf(self, out, in_, func, bias=None, scale=None, accum_out=None, r=(), w=()):
        nc = self.nc
        kw = {}
        if bias is not None:
            kw["bias"] = bias
        if scale is not None:
            kw["scale"] = scale
        if accum_out is not None:
            kw["accum_out"] = accum_out
        return self.fw.op(self.fw.act, lambda: nc.scalar.activation(out=out, in_=in_, func=func, **kw), reads=r, writes=w)

    def red(self, e, out, in_, op, r=(), w=()):
        eng, h = self._eng(e)
        return self.fw.op(eng, lambda: h.tensor_reduce(out=out, in_=in_, axis=AX.X, op=op), reads=r, writes=w)

    def memset(self, e, ap, val, w=()):
        eng, h = self._eng(e)
        return self.fw.op(eng, lambda: h.memset(ap, val), writes=w)

    def recip(self, out, in_, r=(), w=()):
        nc = self.nc
        return self.fw.op(self.fw.dve, lambda: nc.vector.reciprocal(out=out, in_=in_), reads=r, writes=w)

    def rsq(self, out, in_, mul, add, r=(), w=()):
        self.actf(out, in_, AF.Ln, bias=add, scale=mul, r=r, w=w)
        return self.actf(out, out, AF.Exp, scale=-0.5, r=list(r) + list(w), w=w)

    def dma(self, out, in_, r=(), w=(), q="sp"):
        nc = self.nc
        if q == "sp":
            return self.fw.dma(self.fw.sp, lambda: nc.sync.dma_start(out=out, in_=in_), reads=r, writes=w)
        return self.fw.dma(self.fw.pool, lambda: nc.gpsimd.dma_start(out=out, in_=in_), reads=r, writes=w)


def _v3(ap, a):
    return ap.rearrange("p (a b) -> p a b", a=a)


def build(NSEQ=2, S=2048, debug=False, stop_after=None):
    nc = bass.Bass("TRN2", target_bir_lowering=False)
    _BARRIER.clear()
    NTOK = NSEQ * S
    NUB = S // UT
    NTT = S // 128

    def din(name, shape, dt=F32):
        return nc.dram_tensor(name, list(shape), dt, kind="ExternalInput").ap()

    x_d = din("x", [NTOK, D])
    cT_d = din("cT", [128, 8, NSEQ])
    w_ada_d = din("w_ada", [D, 6 * D])
    b_ada_col_d = din("b_ada_col", [128, 48])
    b_ada_row_d = din("b_ada_row", [1, 6 * D])
    ncols_d = din("ncols", [128, 16])
    w_in_d = din("w_in", [D, 5376])
    mu_d = din("mu_cols", [128, 15])
    rwc_d = din("rw_cols", [128, 28])
    wup_d = din("w_up", [64, 512])
    aup_d = din("a_up", [64, 512])
    gup_d = din("g_up", [128, 512])
    qkg_d = din("qkg", [128, 2])
    btab_d = din("btab", [128, 8, 640])
    bmask_d = din("bmask", [128, 640])
    wbr_d = din("w_br", [512, D])
    wba_d = din("w_ba", [512, D])
    wout_d = din("w_out", [D, D])
    wr_d = din("w_router", [D, 36])
    br_d = din("b_router", [1, 36])
    eg_d = din("e_gate", [NEXP, D, DEXP])
    eu_d = din("e_up", [NEXP, D, DEXP])
    ed_d = din("e_down", [NEXP, DEXP, D])
    cst_d = din("consts", [128, 1280])
    out_d = nc.dram_tensor("out", [NTOK, D], F32, kind="ExternalOutput").ap()
    x1_d = nc.dram_tensor("x1s", [NTOK, D], F32, kind="Internal").ap()
    xbuf_d = [nc.dram_tensor(f"xbuf{b}", [NEXP * 256, D], BF16, kind="Internal").ap() for b in range(NSEQ)]
    ybuf_d = [nc.dram_tensor(f"ybuf{b}", [NEXP * 256, D], F32, kind="Internal").ap() for b in range(NSEQ)]
    dbg = {}
    if debug:
        dbg["yr"] = nc.dram_tensor("dbg_yr", [512, NTOK], F32, kind="ExternalOutput").ap()
        dbg["oa"] = nc.dram_tensor("dbg_oa", [512, NTOK], F32, kind="ExternalOutput").ap()
        dbg["h"] = nc.dram_tensor("dbg_h", [D, NTOK], F32, kind="ExternalOutput").ap()

    k = K(nc)
    k.init_psum()
    finals = []
    x1_bufs = {}

    cst = k.sb("cst", [128, 1280])
    b_cst = Buf()
    k.dma(cst[:], cst_d, w=[b_cst])
    ident = cst[:, 0:128]
    blockones = cst[:, 128:256]
    maskAT4 = cst[:, 256:768]
    maskA = cst[:, 768:896]
    II = cst[:, 896:960]
    identb = k.sb("identb", [128, 128], BF16)
    blockonesb = k.sb("blockonesb", [128, 128], BF16)
    b_cb = Buf()
    k.cp("dve", identb[:], ident, r=[b_cst], w=[b_cb])
    k.cp("dve", blockonesb[:], blockones, r=[b_cst], w=[b_cb])

    ncols = k.sb("ncols", [128, 16])
    mu = k.sb("mu", [128, 15])
    omu = k.sb("omu", [128, 15])
    rwc = k.sb("rwc", [128, 28])
    qkg = k.sb("qkg", [128, 2])
    badac = k.sb("badac", [128, 48])
    b_par = Buf()
    k.dma(ncols[:], ncols_d, w=[b_par])
    k.dma(mu[:], mu_d, w=[b_par])
    k.dma(rwc[:], rwc_d, w=[b_par])
    k.dma(qkg[:], qkg_d, w=[b_par])
    k.dma(badac[:], b_ada_col_d, w=[b_par])
    b_par2 = Buf()
    k.ts("dve", omu[:], mu[:], -1.0, 1.0, ALU.mult, ALU.add, r=[b_par], w=[b_par2])
    k.ts("dve", qkg[:, 0:1], qkg[:, 0:1], 0.125, None, ALU.mult, r=[b_par], w=[b_par])
    W0, A0, KK_, KA, RK, LG, LB = [rwc[:, 4 * i:4 * i + 4] for i in range(7)]

    cT = k.sb("cT", [128, 8, NSEQ])
    scb = k.sb("scb", [128, 8, NSEQ], BF16)
    screp = k.sb("screp", [128, 8, NSEQ, 128], BF16)
    modc = k.sb("modc", [128, 4, 8, NSEQ])
    AB = k.sb("AB", [128, 4, 8, NSEQ])
    b_sc = Buf()
    b_modc = Buf()
    b_AB = Buf()
    k.dma(cT[:], cT_d, w=[b_sc])
    k.actf(scb[:], cT[:], AF.Silu, r=[b_sc], w=[b_sc])
    for b in range(NSEQ):
        k.cp("dve", screp[:, :, b, :], scb[:, :, b:b + 1].to_broadcast([128, 8, 128]), r=[b_sc], w=[b_sc])
    m_mod = k.mark()
    wa = [k.sb(f"wa{i}", [128, 8, 1024], BF16) for i in range(2)]
    b_wa = [Buf(), Buf()]
    for vi, blk in enumerate((0, 1, 3, 4)):
        wt, bw = wa[vi % 2], b_wa[vi % 2]
        k.dma(wt[:], w_ada_d[:, blk * 1024:(blk + 1) * 1024].rearrange("(kt p) c -> p kt c", p=128), w=[bw], q="pool")
        ps, bp = k.bank()
        for ct in range(8):
            for kt in range(8):
                k.mm(ps[:, ct * NSEQ:(ct + 1) * NSEQ], wt[:, kt, ct * 128:(ct + 1) * 128], scb[:, kt, :],
                     start=(kt == 0), stop=(kt == 7), r=[bw, b_sc], w=[bp], inc=(ct == 7 and kt == 7))
        k.tt("dve", modc[:, vi, :, :], _v3(ps[:, 0:8 * NSEQ], 8),
             badac[:, blk * 8:(blk + 1) * 8].unsqueeze(2).to_broadcast([128, 8, NSEQ]), ALU.add,
             r=[bp, b_par], w=[b_modc])
    for half in range(2):
        gcol = ncols[:, half * 8:(half + 1) * 8].unsqueeze(2).to_broadcast([128, 8, NSEQ])
        k.stt("dve", AB[:, 2 * half, :, :], modc[:, 2 * half + 1, :, :], 1.0, gcol, ALU.add, ALU.mult,
              r=[b_modc, b_par], w=[b_AB])
        k.cp("dve", AB[:, 2 * half + 1, :, :], modc[:, 2 * half, :, :], r=[b_modc], w=[b_AB])
    k.release(m_mod)

    def norm_to_fm(xt, bx, hsel, seq, outs, tmp):
        ss, sq, xn, rstd, b_t = tmp
        k.actf(sq[:], xt, AF.Square, accum_out=ss[:, 0:1], r=[bx], w=[b_t])
        k.rsq(rstd[:, 0:1], ss[:, 0:1], 1.0 / D, NORM_EPS, r=[b_t], w=[b_t])
        k.actf(xn[:], xt, AF.Copy, scale=rstd[:, 0:1], r=[bx, b_t], w=[b_t])
        for half in range(2):
            ps, bp = k.bank()
            for j in range(4):
                kt = half * 4 + j
                k.mm(ps[:, j * 128:(j + 1) * 128], xn[:, kt * 128:(kt + 1) * 128], ident if xn.dtype == F32 else identb[:],
                     r=[b_t, b_cst, b_cb], w=[bp], inc=(j == 3))
            for j in range(4):
                kt = half * 4 + j
                for oi, (dst, bd) in enumerate(outs):
                    eng = "act" if half == 0 else "dve"
                    if eng == "act":
                        k.actf(dst[:, kt, :], ps[:, j * 128:(j + 1) * 128], AF.Identity,
                               bias=AB[:, 2 * hsel + 1, kt, seq:seq + 1], scale=AB[:, 2 * hsel, kt, seq:seq + 1],
                               r=[bp, b_AB], w=[bd])
                    else:
                        k.ts("dve", dst[:, kt, :], ps[:, j * 128:(j + 1) * 128],
                             AB[:, 2 * hsel, kt, seq:seq + 1], AB[:, 2 * hsel + 1, kt, seq:seq + 1], ALU.mult, ALU.add,
                             r=[bp, b_AB], w=[bd])

    m_seq = k.mark()
    for seq in range(NSEQ):
        k.release(m_seq)
        tok0 = seq * S
        hT = k.sb("hT", [128, 8, S], BF16)
        b_hT = Buf()
        yrT = k.sb("yrT", [128, 4, S], BF16)
        b_yrT = Buf()

        mA = k.mark()
        xts = [k.sb(f"xt{i}", [128, D]) for i in range(2)]
        bxs = [Buf(), Buf()]
        tmpA = []
        for i in range(2):
            tmpA.append((k.sb(f"ssA{i}", [128, 1]), k.sb(f"sqA{i}", [128, D], BF16), k.sb(f"xnA{i}", [128, D], BF16),
                         k.sb(f"rstdA{i}", [128, 1]), Buf()))
        for tt in range(NTT):
            xt, bx = xts[tt % 2], bxs[tt % 2]
            k.dma(xt[:], x_d[tok0 + tt * 128: tok0 + (tt + 1) * 128, :], w=[bx])
            k.memset("pool", tmpA[tt % 2][0][:], 0.0, w=[tmpA[tt % 2][4]])
            norm_to_fm(xt[:], bx, 0, seq, [(hT[:, :, tt * 128:(tt + 1) * 128], b_hT)], tmpA[tt % 2])
        k.release(mA)
        if debug:
            mD = k.mark()
            hf = k.sb("hf_dbg", [128, 8, S])
            bdbg = Buf()
            k.cp("dve", hf[:], hT[:], r=[b_hT], w=[bdbg])
            finals.append(k.dma(dbg["h"][:, tok0:tok0 + S].rearrange("(kt p) t -> p kt t", p=128), hf[:], r=[bdbg], w=[Buf()]))
            k.release(mD)

        mR = k.mark()
        if stop_after != "A":
            rwkv_branch(k, nc, locals())
        k.release(mR)
        if debug:
            mD = k.mark()
            yf = k.sb("yf_dbg", [128, 4, S])
            bdbg = Buf()
            k.cp("dve", yf[:], yrT[:], r=[b_yrT], w=[bdbg])
            finals.append(k.dma(dbg["yr"][:, tok0:tok0 + S].rearrange("(kt p) t -> p kt t", p=128), yf[:], r=[bdbg], w=[Buf()]))
            k.release(mD)
        if stop_after in ("A", "R"):
            continue

        oT = k.sb("oT", [128, 4, S], BF16)
        b_oT = Buf()
        mAt = k.mark()
        attention_branch(k, nc, locals())
        k.release(mAt)
        if debug:
            mD = k.mark()
            of = k.sb("of_dbg", [128, 4, S])
            bdbg = Buf()
            k.cp("dve", of[:], oT[:], r=[b_oT], w=[bdbg])
            finals.append(k.dma(dbg["oa"][:, tok0:tok0 + S].rearrange("(kt p) t -> p kt t", p=128), of[:], r=[bdbg], w=[Buf()]))
            k.release(mD)
        if stop_after == "D":
            continue

        mM = k.mark()
        merge_phase(k, nc, locals())
        k.release(mM)
    k.release(m_seq)

    if stop_after is None:
        mE = k.mark()
        moe_phase(k, nc, locals())
        k.release(mE)

    k.fw.emit_all(finals)
    k.release(0)
    for cm in reversed(k._pcm):
        cm.__exit__(None, None, None)
    k.fw.close()
    return nc


import os
RW_STOP = int(os.environ.get("RW_STOP", "99"))
RW_NCH = int(os.environ.get("RW_NCH", "99"))
RW_SUB = int(os.environ.get("RW_SUB", "99"))


def rwkv_branch(k, nc, L):
    S, seq = L["S"], L["seq"]
    hT, b_hT, yrT, b_yrT = L["hT"], L["b_hT"], L["yrT"], L["b_yrT"]
    w_in_d, mu, omu, b_par, b_par2 = L["w_in_d"], L["mu"], L["omu"], L["b_par"], L["b_par2"]
    W0, A0, KK_, KA, RK, LG, LB = L["W0"], L["A0"], L["KK_"], L["KA"], L["RK"], L["LG"], L["LB"]
    ident, blockones, maskAT4, maskA, II, b_cst = L["ident"], L["blockones"], L["maskAT4"], L["maskA"], L["II"], L["b_cst"]
    NUB = S // UT
    NCK = UT // CH
    GC = 4
    NG = NCK // GC

    wrw = k.sb("wrw", [128, 8, 1792], BF16)
    b_wrw = Buf()
    for i in range(7):
        k.dma(wrw[:, :, i * 256:(i + 1) * 256],
              w_in_d[:, i * 256:(i + 1) * 256].rearrange("(kt p) c -> p kt c", p=128), w=[b_wrw], q="pool")
    wup = k.sb("wup", [64, 512], BF16)
    aup = k.sb("aup", [64, 512], BF16)
    gup = k.sb("gup", [128, 512], BF16)
    b_lw = Buf()
    k.dma(wup[:], L["wup_d"], w=[b_lw], q="pool")
    k.dma(aup[:], L["aup_d"], w=[b_lw], q="pool")
    k.dma(gup[:], L["gup_d"], w=[b_lw], q="pool")

    carry = k.sb("carry", [128, 15])
    b_carry = Buf()
    k.memset("pool", carry[:], 0.0, w=[b_carry])
    Tst = k.sb("Tst", [128, 4, 64])
    b_Tst = [Buf() for _ in range(4)]
    k.memset("pool", Tst[:], 0.0, w=b_Tst)

    twd = k.sb("twd", [64, UT], BF16)
    tad = k.sb("tad", [64, UT], BF16)
    sgd = k.sb("sgd", [128, UT], BF16)
    b_sh = Buf()
    praw = [k.sb(f"praw{i}", [128, UT + 1]) for i in range(2)]
    b_praw = [Buf(), Buf()]
    pctr = [0]
    T = [k.sb(f"T{i}", [128, UT]) for i in range(6)]
    bT = [Buf() for _ in range(6)]
    AR = k.sb("AR", [128, NCK, 256])
    BK = k.sb("BK", [128, NCK, 256])
    VX = k.sb("VX", [128, NCK, 128])
    b_AR, b_BK, b_VX = Buf(), Buf(), Buf()
    k.memset("pool", AR[:], 0.0, w=[b_AR])
    k.memset("pool", BK[:], 0.0, w=[b_BK])
    k.memset("pool", VX[:], 0.0, w=[b_VX])
    WL = k.sb("WL", [128, NCK])
    b_WL = Buf()
    AT = k.sb("AT", [128, GC, 512])
    b_ATg = Buf()
    PP = [k.sb(f"PP{i}", [128, GC, 256]) for i in range(2)]
    b_PP = [Buf(), Buf()]
    MT = k.sb("MT", [128, GC, 128])
    b_MT = Buf()
    TOK = k.sb("TOK", [128, GC, 448])
    b_TOK = Buf()
    AXp = k.sb("AXp", [128, GC, 128])
    b_AXp = Buf()
    k.memset("pool", AXp[:], 0.0, w=[b_AXp])
    Vp = k.sb("Vp", [128, GC, 64])
    b_Vp = Buf()
    G0T = k.sb("G0T", [128, GC, 128])
    b_G0T = Buf()
    Hh = k.sb("Hh", [128, GC, 64])
    b_H = Buf()
    RpT = k.sb("RpT", [128, GC, 128])
    b_RpT = Buf()
    TS = k.sb("TS", [128, GC + 1, 64])
    b_TS = Buf()
    ysb = k.sb("ysb", [128, GC, 64])
    ysq = k.sb("ysq", [128, GC, 64])
    yst = k.sb("yst", [128, 4, GC])
    b_y = Buf()
    YX = k.sb("YX", [128, GC, 128])
    b_YX = Buf()
    k.memset("pool", YX[:], 0.0, w=[b_YX])
    yfm = k.sb("yfm", [128, UT])
    b_yfm = Buf()

    def project(ct_cols, M, mu_col, ub, dst, bdst, carry_idx):
        pr, bpr = praw[pctr[0] % 2], b_praw[pctr[0] % 2]
        pctr[0] += 1
        ps, bp = k.bank()
        for kt in range(8):
            k.mm(ps[0:M, :], wrw[:, kt, ct_cols:ct_cols + M], hT[:, kt, ub * UT:(ub + 1) * UT],
                 start=(kt == 0), stop=(kt == 7), r=[b_wrw, b_hT], w=[bp], inc=(kt == 7))
        k.cp("pool", pr[0:M, 0:1], carry[0:M, carry_idx:carry_idx + 1], r=[b_carry], w=[bpr])
        k.cp("act", pr[0:M, 1:UT + 1], ps[0:M, :], r=[bp], w=[bpr])
        k.cp("pool", carry[0:M, carry_idx:carry_idx + 1], pr[0:M, UT:UT + 1], r=[bpr], w=[b_carry])
        k.ts("dve", dst, pr[0:M, 1:UT + 1], omu[0:M, mu_col:mu_col + 1], None, ALU.mult, r=[bpr, b_par2], w=[bdst])
        k.stt("dve", dst, pr[0:M, 0:UT], mu[0:M, mu_col:mu_col + 1], dst, ALU.mult, ALU.add, r=[bpr, b_par], w=[bdst])

    if RW_STOP <= 1:
        return
    for ub in range(NUB):
        tsl = slice(ub * UT, (ub + 1) * UT)
        project(1536, 64, 12, ub, T[0][0:64, :], bT[0], 12)
        k.actf(twd[:], T[0][0:64, :], AF.Tanh, r=[bT[0]], w=[b_sh])
        project(1600, 64, 13, ub, T[1][0:64, :], bT[1], 13)
        k.cp("dve", tad[:], T[1][0:64, :], r=[bT[1]], w=[b_sh])
        project(1664, 128, 14, ub, T[2][:], bT[2], 14)
        k.actf(sgd[:], T[2][:], AF.Sigmoid, r=[bT[2]], w=[b_sh])

        if RW_STOP <= 2:
            return
        for hp in range(4):
            c4 = slice(hp, hp + 1)
            hcol = slice(hp * 128, (hp + 1) * 128)
            ps, bp = k.bank()
            k.mm(ps[:, :], wup[:, hcol], twd[:], r=[b_lw, b_sh], w=[bp])
            k.actf(T[0][:], ps[:, :], AF.Sigmoid, bias=W0[:, c4], r=[bp, b_par], w=[bT[0]])
            k.ts("dve", T[0][:], T[0][:], -0.6065306597126334, None, ALU.mult, r=[bT[0]], w=[bT[0]])
            src, dst, bs, bd = T[0], T[1], bT[0], bT[1]
            sh = 1
            while sh < CH:
                s3, d3 = _v3(src[:], NCK), _v3(dst[:], NCK)
                k.cp("pool", d3[:, :, 0:sh], s3[:, :, 0:sh], r=[bs], w=[bd])
                k.tt("dve", d3[:, :, sh:CH], s3[:, :, sh:CH], s3[:, :, 0:CH - sh], ALU.add, r=[bs], w=[bd])
                src, dst, bs, bd = dst, src, bd, bs
                sh *= 2
            k.actf(T[1][:], T[0][:], AF.Exp, r=[bT[0]], w=[bT[1]])
            k.actf(T[0][:], T[0][:], AF.Exp, scale=-1.0, r=[bT[0]], w=[bT[0]])
            Wt, Winv, bW, bWinv = T[1], T[0], bT[1], bT[0]
            k.cp("pool", WL[:], _v3(Wt[:], NCK)[:, :, CH - 1], r=[bW], w=[b_WL])
            ps, bp = k.bank()
            k.mm(ps[:, :], aup[:, hcol], tad[:], r=[b_lw, b_sh], w=[bp])
            k.actf(T[2][:], ps[:, :], AF.Sigmoid, bias=A0[:, c4], r=[bp, b_par], w=[bT[2]])
            project(512 + hp * 128, 128, 4 + hp, ub, T[3][:], bT[3], 4 + hp)
            k.ts("dve", T[4][:], T[3][:], KK_[:, c4], None, ALU.mult, r=[bT[3], b_par], w=[bT[4]])
            k.tt("pool", T[5][:], T[4][:], T[4][:], ALU.mult, r=[bT[4]], w=[bT[5]])
            ps, bp = k.bank()
            k.mm(ps[:, :], blockones, T[5][:], r=[b_cst, bT[5]], w=[bp])
            k.rsq(T[5][:], ps[:, :], 1.0, 1e-12, r=[bp], w=[bT[5]])
            k.tt("dve", T[4][:], T[4][:], T[5][:], ALU.mult, r=[bT[4], bT[5]], w=[bT[4]])
            k.ts("dve", T[5][:], T[2][:], -1.0, KA[:, c4], ALU.add, ALU.mult, r=[bT[2], b_par], w=[bT[5]])
            k.stt("dve", T[3][:], T[5][:], 1.0, T[3][:], ALU.add, ALU.mult, r=[bT[5], bT[3]], w=[bT[3]])
            k.tt("pool", T[5][:], T[4][:], T[2][:], ALU.mult, r=[bT[4], bT[2]], w=[bT[5]])
            for h2 in range(2):
                pr_ = slice(h2 * 64, (h2 + 1) * 64)
                k.tt("dve", BK[pr_, :, h2 * 64:(h2 + 1) * 64], _v3(T[5][pr_, :], NCK), _v3(Winv[pr_, :], NCK), ALU.mult,
                     r=[bT[5], bWinv], w=[b_BK])
                k.tt("dve", BK[pr_, :, 128 + h2 * 64:128 + (h2 + 1) * 64], _v3(T[3][pr_, :], NCK), _v3(Winv[pr_, :], NCK),
                     ALU.mult, r=[bT[3], bWinv], w=[b_BK])
                kk3, W3 = _v3(T[4][pr_, :], NCK), _v3(Wt[pr_, :], NCK)
                k.stt("dve", AR[pr_, :, h2 * 64 + 1:(h2 + 1) * 64], kk3[:, :, 1:CH], -1.0, W3[:, :, 0:CH - 1],
                      ALU.mult, ALU.mult, r=[bT[4], bW], w=[b_AR])
                k.ts("dve", AR[pr_, :, h2 * 64:h2 * 64 + 1], kk3[:, :, 0:1], -1.0, None, ALU.mult, r=[bT[4]], w=[b_AR])
            project(hp * 128, 128, hp, ub, T[2][:], bT[2], hp)
            for h2 in range(2):
                pr_ = slice(h2 * 64, (h2 + 1) * 64)
                k.tt("dve", AR[pr_, :, 128 + h2 * 64:128 + (h2 + 1) * 64], _v3(T[2][pr_, :], NCK), _v3(Wt[pr_, :], NCK),
                     ALU.mult, r=[bT[2], bW], w=[b_AR])
            k.stt("dve", T[4][:], T[2][:], RK[:, c4], T[3][:], ALU.mult, ALU.mult, r=[bT[2], bT[3], b_par], w=[bT[4]])
            psb, bpb = k.bank()
            k.mm(psb[:, :], blockones, T[4][:], r=[b_cst, bT[4]], w=[bpb])
            project(1024 + hp * 128, 128, 8 + hp, ub, T[3][:], bT[3], 8 + hp)
            k.tt("dve", T[4][:], psb[:, :], T[3][:], ALU.mult, r=[bpb, bT[3]], w=[bT[4]])
            for h2 in range(2):
                pr_ = slice(h2 * 64, (h2 + 1) * 64)
                k.cp("pool", VX[pr_, :, h2 * 64:(h2 + 1) * 64], _v3(T[3][pr_, :], NCK), r=[bT[3]], w=[b_VX])
            ps, bp = k.bank()
            k.mm(ps[:, :], gup[:, hcol], sgd[:], r=[b_lw, b_sh], w=[bp])
            k.cp("act", T[5][:], ps[:, :], r=[bp], w=[bT[5]])
            bonus, gT, b_bonus, b_g = T[4], T[5], bT[4], bT[5]

            if RW_STOP <= 3:
                return
            for g in range(NG):
                c0 = g * GC
                for ci in range(GC):
                    c = c0 + ci
                    if ci >= RW_NCH:
                        return
                    psA, bpA = k.bank()
                    k.mm(psA[:, 0:256], BK[:, c, 0:128], AR[:, c, :], r=[b_BK, b_AR], w=[bpA], inc=False)
                    k.mm(psA[:, 256:512], BK[:, c, 128:256], AR[:, c, :], r=[b_BK, b_AR], w=[bpA])
                    k.tt("dve", AT[:, ci, :], psA[:, :], maskAT4, ALU.mult, r=[bpA, b_cst], w=[b_ATg])
                    if RW_SUB <= 0:
                        continue
                    psB, bpB = k.bank()
                    k.mm(psB[:, 0:128], AR[:, c, 0:128], BK[:, c, 0:128], r=[b_AR, b_BK], w=[bpB], inc=False)
                    k.mm(psB[:, 128:192], AR[:, c, 0:128], II, r=[b_AR, b_cst], w=[bpB], inc=False)
                    k.mm(psB[:, 192:256], VX[:, c, :], II, r=[b_VX, b_cst], w=[bpB], inc=False)
                    k.mm(psB[:, 256:384], BK[:, c, 0:128], ident, r=[b_BK, b_cst], w=[bpB], inc=False)
                    k.mm(psB[:, 384:512], BK[:, c, 128:256], ident, r=[b_BK, b_cst], w=[bpB])
                    k.tt("dve", PP[0][:, ci, 0:128], psB[:, 0:128], maskA, ALU.mult, r=[bpB, b_cst], w=[b_PP[0]])
                    k.cp("dve", TOK[:, ci, 0:64], psB[:, 128:192], r=[bpB], w=[b_TOK])
                    k.cp("dve", TOK[:, ci, 128:448], psB[:, 192:512], r=[bpB], w=[b_TOK])
                if RW_SUB <= 1:
                    return
                k.cp("pool", PP[0][:, :, 128:256], AT[:, :, 0:128], r=[b_ATg], w=[b_PP[0]])
                k.tt("pool", MT[:], AT[:, :, 0:128], ident.unsqueeze(1).to_broadcast([128, GC, 128]), ALU.add,
                     r=[b_ATg, b_cst], w=[b_MT])
                if RW_STOP <= 4:
                    return
                cur = 0
                for lev in range(1, 6):
                    Pc, bPc = PP[cur], b_PP[cur]
                    Pn, bPn = PP[1 - cur], b_PP[1 - cur]
                    for half in range(2):
                        ps, bp = k.bank()
                        for cj in range(2):
                            ci = half * 2 + cj
                            k.mm(ps[:, cj * 256:cj * 256 + 128], Pc[:, ci, 128:256], Pc[:, ci, 0:128], r=[bPc], w=[bp], inc=False)
                            k.mm(ps[:, cj * 256 + 128:cj * 256 + 256], Pc[:, ci, 0:128], Pc[:, ci, 128:256], r=[bPc], w=[bp],
                                 inc=(cj == 1))
                        k.cp("act", Pn[:, half * 2:half * 2 + 2, :], _v3(ps[:, :], 2), r=[bp], w=[bPn])
                    ps, bp = k.bank()
                    for ci in range(GC):
                        k.mm(ps[:, ci * 128:(ci + 1) * 128], Pn[:, ci, 0:128], MT[:, ci, :], r=[bPn, b_MT], w=[bp], inc=(ci == GC - 1))
                    k.tt("dve", MT[:], MT[:], _v3(ps[:, :], GC), ALU.add, r=[bp, b_MT], w=[b_MT])
                    cur = 1 - cur
                if RW_STOP <= 5:
                    return
                ps, bp = k.bank()
                for ci in range(GC):
                    k.mm(ps[:, ci * 64:(ci + 1) * 64], AT[:, ci, 256:384], TOK[:, ci, 128:192], r=[b_ATg, b_TOK], w=[bp], inc=(ci == GC - 1))
                k.cp("act", TOK[:, :, 64:128], _v3(ps[:, 0:GC * 64], GC), r=[bp], w=[b_TOK])
                ps, bp = k.bank()
                for ci in range(GC):
                    k.mm(ps[:, ci * 128:(ci + 1) * 128], MT[:, ci, :], TOK[:, ci, 0:128], r=[b_MT, b_TOK], w=[bp], inc=(ci == GC - 1))
                p3 = _v3(ps[:, :], GC)
                k.cp("act", AXp[0:64, :, 0:64], p3[0:64, :, 0:64], r=[bp], w=[b_AXp])
                k.cp("act", AXp[64:128, :, 64:128], p3[64:128, :, 0:64], r=[bp], w=[b_AXp])
                k.cp("act", Vp[:], p3[:, :, 64:128], r=[bp], w=[b_Vp])
                psG, bpG = k.bank()
                psR, bpR = k.bank()
                psH, bpH = k.bank()
                for ci in range(GC):
                    last = (ci == GC - 1)
                    k.mm(psG[:, ci * 128:(ci + 1) * 128], AXp[:, ci, :], TOK[:, ci, 192:320], r=[b_AXp, b_TOK], w=[bpG], inc=last)
                    k.mm(psR[:, ci * 128:(ci + 1) * 128], AXp[:, ci, :], AT[:, ci, 128:256], r=[b_AXp, b_ATg], w=[bpR], inc=last)
                    k.mm(psH[:, ci * 64:(ci + 1) * 64], TOK[:, ci, 192:320], Vp[:, ci, :], start=True, stop=False,
                         r=[b_TOK, b_Vp], w=[bpH], inc=False)
                    k.mm(psH[:, ci * 64:(ci + 1) * 64], TOK[:, ci, 320:448], TOK[:, ci, 128:192], start=False, stop=True,
                         r=[b_TOK], w=[bpH], inc=last)
                k.tt("dve", G0T[:], _v3(psG[:, :], GC), ident.unsqueeze(1).to_broadcast([128, GC, 128]), ALU.add,
                     r=[bpG, b_cst], w=[b_G0T])
                k.tt("dve", RpT[:], _v3(psR[:, :], GC), AR[:, c0:c0 + GC, 128:256], ALU.add, r=[bpR, b_AR], w=[b_RpT])
                k.tt("dve", Hh[:], _v3(psH[:, 0:GC * 64], GC), WL[:, c0:c0 + GC].unsqueeze(2).to_broadcast([128, GC, 64]),
                     ALU.mult, r=[bpH, b_WL], w=[b_H])
                if RW_STOP <= 6:
                    return
                k.cp("pool", TS[:, 0, :], Tst[:, hp, :], r=[b_Tst[hp]], w=[b_TS])
                for ci in range(GC):
                    c = c0 + ci
                    ps, bp = k.bank()
                    k.mm(ps[:, 0:64], G0T[:, ci, :], TS[:, ci, :], r=[b_G0T, b_TS], w=[bp])
                    k.stt("dve", TS[:, ci + 1, :], ps[:, 0:64], WL[:, c:c + 1], Hh[:, ci, :], ALU.mult, ALU.add,
                          r=[bp, b_WL, b_H], w=[b_TS])
                k.cp("pool", Tst[:, hp, :], TS[:, GC, :], r=[b_TS], w=[b_Tst[hp]])
                if RW_STOP <= 7:
                    return
                ps, bp = k.bank()
                for ci in range(GC):
                    o = ps[:, ci * 64:(ci + 1) * 64]
                    k.mm(o, AT[:, ci, 128:256], Vp[:, ci, :], start=True, stop=False, r=[b_ATg, b_Vp], w=[bp], inc=False)
                    k.mm(o, AT[:, ci, 384:512], TOK[:, ci, 128:192], start=False, stop=False, r=[b_ATg, b_TOK], w=[bp], inc=False)
                    k.mm(o, RpT[:, ci, :], TS[:, ci, :], start=False, stop=True, r=[b_RpT, b_TS], w=[bp], inc=(ci == GC - 1))
                k.cp("act", ysb[:], _v3(ps[:, 0:GC * 64], GC), r=[bp], w=[b_y])
                k.red("dve", yst[:, 0, :], ysb[:], ALU.add, r=[b_y], w=[b_y])
                k.tt("pool", ysq[:], ysb[:], ysb[:], ALU.mult, r=[b_y], w=[b_y])
                k.red("dve", yst[:, 1, :], ysq[:], ALU.add, r=[b_y], w=[b_y])
                k.ts("dve", yst[:, 2, :], yst[:, 0, :], 1.0 / 64, None, ALU.mult, r=[b_y], w=[b_y])
                k.tt("dve", yst[:, 3, :], yst[:, 2, :], yst[:, 2, :], ALU.mult, r=[b_y], w=[b_y])
                k.stt("dve", yst[:, 3, :], yst[:, 1, :], 1.0 / 64, yst[:, 3, :], ALU.mult, ALU.subtract, r=[b_y], w=[b_y])
                k.rsq(yst[:, 3, :], yst[:, 3, :], 1.0, GN_EPS, r=[b_y], w=[b_y])
                k.tt("dve", ysb[:], ysb[:], yst[:, 2, :].unsqueeze(2).to_broadcast([128, GC, 64]), ALU.subtract, r=[b_y], w=[b_y])
                for h2 in range(2):
                    pr_ = slice(h2 * 64, (h2 + 1) * 64)
                    k.tt("dve", YX[pr_, :, h2 * 64:(h2 + 1) * 64], ysb[pr_, :, :],
                         yst[pr_, 3, :].unsqueeze(2).to_broadcast([64, GC, 64]), ALU.mult, r=[b_y], w=[b_YX])
                ps, bp = k.bank()
                for ci in range(GC):
                    k.mm(ps[:, ci * 64:(ci + 1) * 64], YX[:, ci, :], II, r=[b_YX, b_cst], w=[bp], inc=(ci == GC - 1))
                k.actf(yfm[:, c0 * CH:(c0 + GC) * CH], ps[:, 0:GC * 64], AF.Identity, bias=LB[:, c4], scale=LG[:, c4],
                       r=[bp, b_par], w=[b_yfm])
            k.tt("dve", yfm[:], yfm[:], bonus[:], ALU.add, r=[b_yfm, b_bonus], w=[b_yfm])
            k.tt("dve", yrT[:, hp, tsl], yfm[:], gT[:], ALU.mult, r=[b_yfm, b_g], w=[b_yrT])


def attention_branch(k, nc, L):
    S, seq = L["S"], L["seq"]
    hT, b_hT, oT, b_oT = L["hT"], L["b_hT"], L["oT"], L["b_oT"]
    w_in_d, qkg, b_par = L["w_in_d"], L["qkg"], L["b_par"]
    identb, blockonesb, b_cb = L["identb"], L["blockonesb"], L["b_cb"]
    NTT = S // 128
    NB = S // 512
    wat = k.sb("wat", [128, 8, 1536], BF16)
    b_wat = Buf()
    for i in range(6):
        k.dma(wat[:, :, i * 256:(i + 1) * 256],
              w_in_d[:, 1792 + i * 256:1792 + (i + 1) * 256].rearrange("(kt p) c -> p kt c", p=128), w=[b_wat], q="pool")
    EB = k.sb("EB", [128, 8, 640], BF16)
    b_EB = Buf()
    bm = k.sb("bm", [128, 640])
    b_bm = Buf()
    k.dma(bm[:], L["bmask_d"], w=[b_bm])
    bst = [k.sb(f"bst{i}", [128, 640]) for i in range(2)]
    b_bst = [Buf(), Buf()]
    for h in range(8):
        t, bt = bst[h % 2], b_bst[h % 2]
        k.dma(t[:], L["btab_d"][:, h, :], w=[bt])
        k.tt("pool", t[:], t[:], bm[:], ALU.add, r=[bt, b_bm], w=[bt])
        k.actf(EB[:, h, :], t[:], AF.Exp, r=[bt], w=[b_EB])

    qT = k.sb("qT", [128, 4, S], BF16)
    kT = k.sb("kT", [128, 4, S], BF16)
    b_qT, b_kT = Buf(), Buf()
    Va = k.sb("Va", [128, NTT, 8, 65], BF16)
    b_Va = Buf()
    k.memset("pool", Va[:], 1.0, w=[b_Va])
    sqb = [k.sb(f"sqb{i}", [128, 512], BF16) for i in range(2)]
    raw = [k.sb(f"qraw{i}", [128, 512]) for i in range(2)]
    rs = [k.sb(f"qrs{i}", [128, 512]) for i in range(2)]
    b_n = [Buf(), Buf()]
    it = 0
    for which in range(2):
        dstT, bdst = (qT, b_qT) if which == 0 else (kT, b_kT)
        for ct in range(4):
            col0 = which * 512 + ct * 128
            for blk in range(NB):
                i = it % 2
                it += 1
                ps, bp = k.bank()
                for kt in range(8):
                    k.mm(ps[:, :], wat[:, kt, col0:col0 + 128], hT[:, kt, blk * 512:(blk + 1) * 512],
                         start=(kt == 0), stop=(kt == 7), r=[b_wat, b_hT], w=[bp], inc=(kt == 7))
                k.cp("dve", raw[i][:], ps[:, :], r=[bp], w=[b_n[i]])
                k.actf(sqb[i][:], raw[i][:], AF.Square, r=[b_n[i]], w=[b_n[i]])
                ps2, bp2 = k.bank()
                k.mm(ps2[:, :], blockonesb[:], sqb[i][:], r=[b_cb, b_n[i]], w=[bp2])
                k.rsq(rs[i][:], ps2[:, :], 1.0 / 64, NORM_EPS, r=[bp2], w=[b_n[i]])
                k.stt("dve", dstT[:, ct, blk * 512:(blk + 1) * 512], raw[i][:], qkg[:, which:which + 1], rs[i][:],
                      ALU.mult, ALU.mult, r=[b_n[i], b_par], w=[bdst])
    for tt in range(NTT):
        ps, bp = k.bank()
        for kt in range(8):
            k.mm(ps[:, :], hT[:, kt, tt * 128:(tt + 1) * 128], wat[:, kt, 1024:1536],
                 start=(kt == 0), stop=(kt == 7), r=[b_wat, b_hT], w=[bp], inc=(kt == 7))
        k.cp("act", Va[:, tt, :, 0:64], _v3(ps[:, :], 8), r=[bp], w=[b_Va])

    PT = [k.sb(f"PT{i}", [128, 640], BF16) for i in range(3)]
    b_PT = [Buf() for _ in range(3)]
    otok = [k.sb(f"otok{i}", [128, 512], BF16) for i in range(2)]
    b_otok = [Buf(), Buf()]
    rec = [k.sb(f"rec{i}", [128, 4]) for i in range(2)]
    b_rec = [Buf(), Buf()]
    pi = 0
    for p in range(NTT):
        j0 = max(0, 4 - p)
        ot, bot = otok[p % 2], b_otok[p % 2]
        for hh in range(2):
            pso_b = k.bank(hold=True)
            pso, bpo = pso_b
            for h4 in range(4):
                h = hh * 4 + h4
                ct, pb = h // 2, (h % 2) * 64
                Pt, bPt = PT[pi % 3], b_PT[pi % 3]
                pi += 1
                ps1, bp1 = k.bank()
                ps2, bp2 = k.bank()
                for j in range(j0, 5):
                    tt = p - 4 + j
                    dst = ps1[:, j * 128:(j + 1) * 128] if j < 4 else ps2[:, 0:128]
                    k.mm(dst, kT[pb:pb + 64, ct, tt * 128:(tt + 1) * 128], qT[pb:pb + 64, ct, p * 128:(p + 1) * 128],
                         r=[b_kT, b_qT], w=[bp1 if j < 4 else bp2], inc=(j == 3 or j == 4))
                if j0 < 4:
                    k.actf(Pt[:, j0 * 128:512], ps1[:, j0 * 128:512], AF.Exp, r=[bp1], w=[bPt])
                k.actf(Pt[:, 512:640], ps2[:, 0:128], AF.Exp, r=[bp2], w=[bPt])
                k.tt("pool", Pt[:, j0 * 128:640], Pt[:, j0 * 128:640], EB[:, h, j0 * 128:640],
                     ALU.mult, r=[bPt, b_EB], w=[bPt])
                for j in range(j0, 5):
                    tt = p - 4 + j
                    k.mm(pso[:, h4 * 65:(h4 + 1) * 65], Pt[:, j * 128:(j + 1) * 128], Va[:, tt, h, :],
                         start=(j == j0), stop=(j == 4), r=[bPt, b_Va], w=[bpo], inc=(j == 4))
            po3 = pso[:, 0:260].rearrange("p (a b) -> p a b", a=4)
            rc, brc = rec[hh], b_rec[hh]
            k.recip(rc[:], po3[:, :, 64], r=[bpo], w=[brc])
            k.tt("dve", _v3(ot[:, hh * 256:(hh + 1) * 256], 4), po3[:, :, 0:64],
                 rc[:].unsqueeze(2).to_broadcast([128, 4, 64]), ALU.mult, r=[bpo, brc], w=[bot])
            k.unhold(pso_b)
        ps, bp = k.bank()
        for ct in range(4):
            k.mm(ps[:, ct * 128:(ct + 1) * 128], ot[:, ct * 128:(ct + 1) * 128], identb[:], r=[bot, b_cb], w=[bp], inc=(ct == 3))
        k.cp("act", oT[:, :, p * 128:(p + 1) * 128], _v3(ps[:, :], 4), r=[bp], w=[b_oT])


def merge_phase(k, nc, L):
    S, seq, tok0 = L["S"], L["seq"], L["tok0"]
    hT, b_hT, oT, b_oT, yrT, b_yrT = L["hT"], L["b_hT"], L["oT"], L["b_oT"], L["yrT"], L["b_yrT"]
    w_in_d, w_ada_d, screp, b_sc = L["w_in_d"], L["w_ada_d"], L["screp"], L["b_sc"]
    x_d, x1_d = L["x_d"], L["x1_d"]
    NB = S // 512
    wg = k.sb("wg", [128, 8, 2048], BF16)
    b_wg = Buf()
    for i in range(8):
        k.dma(wg[:, :, i * 256:(i + 1) * 256],
              w_in_d[:, 3328 + i * 256:3328 + (i + 1) * 256].rearrange("(kt p) c -> p kt c", p=128), w=[b_wg], q="pool")
    wbr = k.sb("wbr", [128, 4, 1024], BF16)
    wba = k.sb("wba", [128, 4, 1024], BF16)
    wo = k.sb("wo", [128, 8, 1024], BF16)
    b_w = Buf()
    k.dma(wbr[:], L["wbr_d"].rearrange("(kt p) c -> p kt c", p=128), w=[b_w], q="pool")
    k.dma(wba[:], L["wba_d"].rearrange("(kt p) c -> p kt c", p=128), w=[b_w], q="pool")
    for i in range(2):
        k.dma(wo[:, :, i * 512:(i + 1) * 512], L["wout_d"][:, i * 512:(i + 1) * 512].rearrange("(kt p) c -> p kt c", p=128),
              w=[b_w], q="pool")
    g1row = k.sb("g1row", [128, 1024])
    b_g1 = Buf()
    mG = k.mark()
    wag = k.sb("wag", [128, 8, 1024], BF16)
    b_wag = Buf()
    brow = k.sb("brow", [128, 1024])
    b_brow = Buf()
    k.dma(wag[:], w_ada_d[:, 2048:3072].rearrange("(kt p) c -> p kt c", p=128), w=[b_wag], q="pool")
    k.dma(brow[:], L["b_ada_row_d"][0:1, 2048:3072].partition_broadcast(128), w=[b_brow])
    for half in range(2):
        ps, bp = k.bank()
        for kt in range(8):
            k.mm(ps[:, :], screp[:, kt, seq, :], wag[:, kt, half * 512:(half + 1) * 512], start=(kt == 0), stop=(kt == 7),
                 r=[b_sc, b_wag], w=[bp], inc=(kt == 7))
        k.tt("dve", g1row[:, half * 512:(half + 1) * 512], ps[:, :], brow[:, half * 512:(half + 1) * 512], ALU.add,
             r=[bp, b_brow], w=[b_g1])
    k.release(mG)

    sg = k.sb("sg", [128, 16, 512], BF16)
    b_sg = Buf()
    mixT = k.sb("mixT", [128, 8, 512], BF16)
    b_mix = Buf()
    t1 = [k.sb(f"mt1_{i}", [128, 512]) for i in range(2)]
    t2 = [k.sb(f"mt2_{i}", [128, 512]) for i in range(2)]
    b_t = [Buf(), Buf()]
    xt = [k.sb(f"mxt{i}", [128, 1024]) for i in range(2)]
    b_xt = [Buf(), Buf()]
    x1t = [k.sb(f"mx1t{i}", [128, 1024]) for i in range(2)]
    b_x1t = [Buf(), Buf()]
    ti = 0
    for blk in range(NB):
        bsl = slice(blk * 512, (blk + 1) * 512)
        for ct in range(16):
            ps, bp = k.bank()
            for kt in range(8):
                k.mm(ps[:, :], wg[:, kt, ct * 128:(ct + 1) * 128], hT[:, kt, bsl], start=(kt == 0), stop=(kt == 7),
                     r=[b_wg, b_hT], w=[bp], inc=(kt == 7))
            k.actf(sg[:, ct, :], ps[:, :], AF.Sigmoid, r=[bp], w=[b_sg])
        for dt_ in range(8):
            i = dt_ % 2
            psr, bpr = k.bank()
            for kt in range(4):
                k.mm(psr[:, :], wbr[:, kt, dt_ * 128:(dt_ + 1) * 128], yrT[:, kt, bsl], start=(kt == 0), stop=(kt == 3),
                     r=[b_w, b_yrT], w=[bpr], inc=(kt == 3))
            psa, bpa = k.bank()
            for kt in range(4):
                k.mm(psa[:, :], wba[:, kt, dt_ * 128:(dt_ + 1) * 128], oT[:, kt, bsl], start=(kt == 0), stop=(kt == 3),
                     r=[b_w, b_oT], w=[bpa], inc=(kt == 3))
            k.tt("dve", t1[i][:], psr[:, :], sg[:, dt_, :], ALU.mult, r=[bpr, b_sg], w=[b_t[i]])
            k.tt("dve", t2[i][:], psa[:, :], sg[:, 8 + dt_, :], ALU.mult, r=[bpa, b_sg], w=[b_t[i]])
            k.tt("pool", mixT[:, dt_, :], t1[i][:], t2[i][:], ALU.add, r=[b_t[i]], w=[b_mix])
        for tq in range(4):
            i = ti % 2
            ti += 1
            r0 = tok0 + blk * 512 + tq * 128
            k.dma(xt[i][:], x_d[r0:r0 + 128, :], w=[b_xt[i]])
            for half in range(2):
                ps, bp = k.bank()
                for kt in range(8):
                    k.mm(ps[:, :], mixT[:, kt, tq * 128:(tq + 1) * 128], wo[:, kt, half * 512:(half + 1) * 512],
                         start=(kt == 0), stop=(kt == 7), r=[b_mix, b_w], w=[bp], inc=(kt == 7))
                hs = slice(half * 512, (half + 1) * 512)
                k.tt("dve", x1t[i][:, hs], ps[:, :], g1row[:, hs], ALU.mult, r=[bp, b_g1], w=[b_x1t[i]])
                k.tt("pool", x1t[i][:, hs], x1t[i][:, hs], xt[i][:, hs], ALU.add, r=[b_x1t[i], b_xt[i]], w=[b_x1t[i]])
            L["x1_bufs"][r0 // 128] = Buf()
            k.dma(x1_d[r0:r0 + 128, :], x1t[i][:], r=[b_x1t[i]], w=[L["x1_bufs"][r0 // 128]])


CAP = 256


def moe_phase(k, nc, L):
    S, NSEQ = L["S"], L["NSEQ"]
    NTT = S // 128
    NT = NSEQ * NTT
    NROWS = NEXP * CAP
    NBLK = NSEQ * (CAP // 128)
    x1_d, out_d = L["x1_d"], L["out_d"]
    xbuf_d, ybuf_d = L["xbuf_d"], L["ybuf_d"]
    ident, identb, b_cst, b_cb = L["ident"], L["identb"], L["b_cst"], L["b_cb"]
    AB, b_AB, screp, b_sc, cst = L["AB"], L["b_AB"], L["screp"], L["b_sc"], L["cst"]
    finals = L["finals"]
    triU = cst[:, 1088:1216]
    basem1 = cst[:, 1216:1216 + NSEQ * 32]
    _bcreg = {}

    def bc_reg():
        if "r" not in _bcreg:
            _bcreg["r"] = nc.gpsimd.to_reg(NROWS - 1)
        return _bcreg["r"]

    trib = k.sb("trib", [128, 128], BF16)
    onesb = k.sb("onesb", [128, 128], BF16)
    b_tb = Buf()
    k.cp("dve", trib[:], triU, r=[b_cst], w=[b_tb])
    k.cp("dve", onesb[:], cst[:, 960:1088], r=[b_cst], w=[b_tb])

    mZ = k.mark()
    zt = k.sb("zt", [128, 8, 1024], BF16)
    b_zt = Buf()
    k.memset("pool", zt[:], 0.0, w=[b_zt])
    b_xz = Buf()
    for sq in range(NSEQ):
        for i in range(NROWS // 1024):
            k.dma(xbuf_d[sq][i * 1024:(i + 1) * 1024, :].rearrange("(n p) d -> p n d", p=128), zt[:], r=[b_zt], w=[b_xz])
    g2row = k.sb("g2row", [128, NSEQ, 1024])
    b_g2 = Buf()
    wag = k.sb("wag2", [128, 8, 1024], BF16)
    b_wag = Buf()
    brow = k.sb("brow2", [128, 1024])
    b_brow = Buf()
    k.dma(wag[:], L["w_ada_d"][:, 5120:6144].rearrange("(kt p) c -> p kt c", p=128), w=[b_wag], q="pool")
    k.dma(brow[:], L["b_ada_row_d"][0:1, 5120:6144].partition_broadcast(128), w=[b_brow])
    for seq in range(NSEQ):
        for half in range(2):
            ps, bp = k.bank()
            for kt in range(8):
                k.mm(ps[:, :], screp[:, kt, seq, :], wag[:, kt, half * 512:(half + 1) * 512], start=(kt == 0), stop=(kt == 7),
                     r=[b_sc, b_wag], w=[bp], inc=(kt == 7))
            k.tt("dve", g2row[:, seq, half * 512:(half + 1) * 512], ps[:, :], brow[:, half * 512:(half + 1) * 512], ALU.add,
                 r=[bp, b_brow], w=[b_g2])
    k.release(mZ)
    g2row_keep = g2row
    g2r = k.sb("g2r", [128, NSEQ, 1024])
    b_g2 = Buf()
    wag = k.sb("wag3", [128, 8, 1024], BF16)
    b_wag = Buf()
    brow = k.sb("brow3", [128, 1024])
    b_brow = Buf()
    k.dma(wag[:], L["w_ada_d"][:, 5120:6144].rearrange("(kt p) c -> p kt c", p=128), w=[b_wag], q="pool")
    k.dma(brow[:], L["b_ada_row_d"][0:1, 5120:6144].partition_broadcast(128), w=[b_brow])
    for seq in range(NSEQ):
        for half in range(2):
            ps, bp = k.bank()
            for kt in range(8):
                k.mm(ps[:, :], screp[:, kt, seq, :], wag[:, kt, half * 512:(half + 1) * 512], start=(kt == 0), stop=(kt == 7),
                     r=[b_sc, b_wag], w=[bp], inc=(kt == 7))
            k.tt("dve", g2r[:, seq, half * 512:(half + 1) * 512], ps[:, :], brow[:, half * 512:(half + 1) * 512], ALU.add,
                 r=[bp, b_brow], w=[b_g2])

    wr = k.sb("wr", [128, 8, 36])
    brt = k.sb("brt", [128, 36])
    b_wr = Buf()
    k.dma(wr[:], L["wr_d"].rearrange("(kt p) c -> p kt c", p=128), w=[b_wr])
    k.dma(brt[:], L["br_d"][0:1, :].partition_broadcast(128), w=[b_wr])

    desti = k.sb("desti", [128, NT, 2], mybir.dt.int32)
    wts = k.sb("wts", [128, NT, 2])
    b_route = [Buf() for _ in range(NT)]
    carry = k.sb("mcarry", [128, NSEQ, 32])
    b_carry = Buf()
    k.memset("pool", carry[:], 0.0, w=[b_carry])
    b_xs = [Buf() for _ in range(NT)]

    mP = k.mark()
    xts = [k.sb(f"ext{i}", [128, D]) for i in range(2)]
    bxs = [Buf(), Buf()]
    tmpA = []
    for i in range(2):
        tmpA.append((k.sb(f"ess{i}", [128, 1]), k.sb(f"esq{i}", [128, D]), k.sb(f"exn{i}", [128, D]),
                     k.sb(f"erstd{i}", [128, 1]), Buf()))
    xnb = [k.sb(f"xnb{i}", [128, D], BF16) for i in range(2)]
    b_xnb = [Buf(), Buf()]
    h2f = [k.sb(f"h2f{i}", [128, 8, 128]) for i in range(2)]
    b_h2f = [Buf(), Buf()]
    lg = k.sb("lg", [128, 36])
    rt = k.sb("rt", [128, 64])
    fs = k.sb("fs", [128, 4, 8])
    m32 = k.sb("m32", [128, 3, 32])
    maskb = k.sb("maskb", [128, 32], BF16)
    rk = k.sb("rk", [128, 4, 32])
    dstf = k.sb("dstf", [128, 2])
    b_rt = Buf()
    for t in range(NT):
        i = t % 2
        seq = t // NTT
        r0 = t * 128
        k.dma(xts[i][:], x1_d[r0:r0 + 128, :], r=[L["x1_bufs"][t]], w=[bxs[i]])
        k.memset("pool", tmpA[i][0][:], 0.0, w=[tmpA[i][4]])
        L["norm_to_fm"](xts[i][:], bxs[i], 1, seq, [(h2f[i][:], b_h2f[i])], tmpA[i])
        k.cp("pool", xnb[i][:], tmpA[i][2][:], r=[tmpA[i][4]], w=[b_xnb[i]])
        ps, bp = k.bank()
        for kt in range(8):
            k.mm(ps[:, 0:36], h2f[i][:, kt, :], wr[:, kt, :], start=(kt == 0), stop=(kt == 7), r=[b_h2f[i], b_wr], w=[bp],
                 inc=(kt == 7))
        k.tt("dve", lg[:], ps[:, 0:36], brt[:], ALU.add, r=[bp, b_wr], w=[b_rt])
        cm, ce, csum, oh = rt[:, 0:1], rt[:, 4:8], rt[:, 1:2], rt[:, 8:12]
        k.red("dve", cm, lg[:, 0:4], ALU.max, r=[b_rt], w=[b_rt])
        k.ts("dve", oh, lg[:, 0:4], cm, None, ALU.is_equal, r=[b_rt], w=[b_rt])
        k.ts("dve", ce, lg[:, 0:4], cm, None, ALU.subtract, r=[b_rt], w=[b_rt])
        k.actf(ce, ce, AF.Exp, r=[b_rt], w=[b_rt])
        k.red("dve", csum, ce, ALU.add, r=[b_rt], w=[b_rt])
        pg = rt[:, 2:3]
        k.recip(pg, csum, r=[b_rt], w=[b_rt])
        k.tt("dve", fs[:], _v3(lg[:, 4:36], 4), oh.unsqueeze(2).to_broadcast([128, 4, 8]), ALU.mult, r=[b_rt], w=[b_rt])
        fsel = rt[:, 16:24]
        k.tt("dve", rt[:, 24:32], fs[:, 0, :], fs[:, 1, :], ALU.add, r=[b_rt], w=[b_rt])
        k.tt("dve", rt[:, 32:40], fs[:, 2, :], fs[:, 3, :], ALU.add, r=[b_rt], w=[b_rt])
        k.tt("dve", fsel, rt[:, 24:32], rt[:, 32:40], ALU.add, r=[b_rt], w=[b_rt])
        m1, m2 = rt[:, 40:41], rt[:, 41:42]
        oh1, oh2 = rt[:, 24:32], rt[:, 32:40]
        k.red("dve", m1, fsel, ALU.max, r=[b_rt], w=[b_rt])
        k.ts("dve", oh1, fsel, m1, None, ALU.is_equal, r=[b_rt], w=[b_rt])
        msk = rt[:, 48:56]
        k.stt("dve", msk, oh1, -1e30, fsel, ALU.mult, ALU.add, r=[b_rt], w=[b_rt])
        k.red("dve", m2, msk, ALU.max, r=[b_rt], w=[b_rt])
        k.ts("dve", oh2, msk, m2, None, ALU.is_equal, r=[b_rt], w=[b_rt])
        e2, den = rt[:, 42:43], rt[:, 43:44]
        k.tt("dve", e2, m2, m1, ALU.subtract, r=[b_rt], w=[b_rt])
        k.actf(e2, e2, AF.Exp, r=[b_rt], w=[b_rt])
        k.ts("dve", den, e2, 1.0, None, ALU.add, r=[b_rt], w=[b_rt])
        k.recip(den, den, r=[b_rt], w=[b_rt])
        k.tt("dve", wts[:, t, 0:1], den, pg, ALU.mult, r=[b_rt], w=[b_route[t]])
        k.tt("dve", wts[:, t, 1:2], wts[:, t, 0:1], e2, ALU.mult, r=[b_rt, b_route[t]], w=[b_route[t]])
        k.tt("dve", _v3(m32[:, 0, :], 4), oh.unsqueeze(2).to_broadcast([128, 4, 8]),
             oh1.unsqueeze(1).to_broadcast([128, 4, 8]), ALU.mult, r=[b_rt], w=[b_rt])
        k.tt("dve", _v3(m32[:, 1, :], 4), oh.unsqueeze(2).to_broadcast([128, 4, 8]),
             oh2.unsqueeze(1).to_broadcast([128, 4, 8]), ALU.mult, r=[b_rt], w=[b_rt])
        k.tt("dve", maskb[:], m32[:, 0, :], m32[:, 1, :], ALU.add, r=[b_rt], w=[b_rt])
        ps, bp = k.bank()
        k.mm(ps[:, 0:32], trib[:], maskb[:], r=[b_tb, b_rt], w=[bp], inc=False)
        k.mm(ps[:, 32:64], onesb[:], maskb[:], r=[b_tb, b_rt], w=[bp])
        k.tt("dve", rk[:, 0, :], ps[:, 0:32], carry[:, seq, :], ALU.add, r=[bp, b_carry], w=[b_rt])
        k.tt("dve", carry[:, seq, :], ps[:, 32:64], carry[:, seq, :], ALU.add, r=[bp, b_carry], w=[b_carry])
        k.ts("dve", rk[:, 1, :], rk[:, 0, :], CAP + 0.5, 1.0e7, ALU.is_gt, ALU.mult, r=[b_rt], w=[b_rt])
        k.tt("dve", rk[:, 1, :], rk[:, 1, :], rk[:, 0, :], ALU.add, r=[b_rt], w=[b_rt])
        k.tt("dve", rk[:, 1, :], rk[:, 1, :], basem1[:, seq * 32:(seq + 1) * 32], ALU.add, r=[b_rt, b_cst], w=[b_rt])
        k.tt("dve", rk[:, 2, :], rk[:, 1, :], m32[:, 0, :], ALU.mult, r=[b_rt], w=[b_rt])
        k.tt("dve", rk[:, 3, :], rk[:, 1, :], m32[:, 1, :], ALU.mult, r=[b_rt], w=[b_rt])
        k.red("dve", dstf[:, 0:2], rk[:, 2:4, :], ALU.add, r=[b_rt], w=[b_rt])
        k.cp("dve", desti[:, t, :], dstf[:, 0:2], r=[b_rt], w=[b_route[t]])
        for c in range(2):
            off = bass.IndirectOffsetOnAxis(ap=desti[:, t, c:c + 1], axis=0)
            xsrc = xnb[i]
            xdst = xbuf_d[seq]
            k.fw.dma(k.fw.pool, (lambda off=off, xsrc=xsrc, xdst=xdst: nc.gpsimd.indirect_dma_start(
                out=xdst[:, :], out_offset=off, in_=xsrc[:, :], in_offset=None,
                bounds_check=bc_reg(), oob_is_err=False)), reads=[b_xnb[i], b_route[t], b_xz], writes=[b_xs[t]])
    k.release(mP)

    mX = k.mark()
    weg = [k.sb(f"weg{i}", [128, 8, 512], BF16) for i in range(2)]
    weu = [k.sb(f"weu{i}", [128, 8, 512], BF16) for i in range(2)]
    wed = [k.sb(f"wed{i}", [128, 4, 1024], BF16) for i in range(2)]
    b_we = [Buf(), Buf()]
    NS = NBLK * 128
    hx = k.sb("hx", [128, 8, NS], BF16)
    b_hx = Buf()
    hid = k.sb("hid", [128, 4, NS], BF16)
    b_hid = Buf()
    sgt = [k.sb(f"sgt{i}", [128, NS]) for i in range(2)]
    b_sgt = [Buf(), Buf()]
    xg = [k.sb(f"xg{i}", [128, 1024], BF16) for i in range(2)]
    b_xg = [Buf(), Buf()]
    yt = [k.sb(f"yt{i}", [128, 1024]) for i in range(2)]
    b_yt = [Buf(), Buf()]
    b_ys = []
    bi = 0
    for e in range(NEXP):
        i = e % 2
        k.dma(weg[i][:], L["eg_d"][e].rearrange("(kt p) c -> p kt c", p=128), w=[b_we[i]], q="pool")
        k.dma(weu[i][:], L["eu_d"][e].rearrange("(kt p) c -> p kt c", p=128), w=[b_we[i]], q="pool")
        k.dma(wed[i][:], L["ed_d"][e].rearrange("(kt p) c -> p kt c", p=128), w=[b_we[i]], q="pool")
        for blk in range(NBLK):
            seq = blk // (CAP // 128)
            r0 = e * CAP + (blk % (CAP // 128)) * 128
            j = bi % 2
            bi += 1
            k.dma(xg[j][:], xbuf_d[seq][r0:r0 + 128, :], r=b_xs + [b_xz], w=[b_xg[j]])
            for half in range(2):
                ps, bp = k.bank()
                for q4 in range(4):
                    kt = half * 4 + q4
                    k.mm(ps[:, q4 * 128:(q4 + 1) * 128], xg[j][:, kt * 128:(kt + 1) * 128], identb[:], r=[b_xg[j], b_cb], w=[bp],
                         inc=(q4 == 3))
                for q4 in range(4):
                    kt = half * 4 + q4
                    if half == 0:
                        k.actf(hx[:, kt, blk * 128:(blk + 1) * 128], ps[:, q4 * 128:(q4 + 1) * 128], AF.Identity,
                               bias=AB[:, 3, kt, seq:seq + 1], scale=AB[:, 2, kt, seq:seq + 1], r=[bp, b_AB], w=[b_hx])
                    else:
                        k.ts("dve", hx[:, kt, blk * 128:(blk + 1) * 128], ps[:, q4 * 128:(q4 + 1) * 128],
                             AB[:, 2, kt, seq:seq + 1], AB[:, 3, kt, seq:seq + 1], ALU.mult, ALU.add, r=[bp, b_AB], w=[b_hx])
        for ht in range(4):
            j = ht % 2
            psg, bpg = k.bank()
            for kt in range(8):
                k.mm(psg[:, 0:NS], weg[i][:, kt, ht * 128:(ht + 1) * 128], hx[:, kt, :], start=(kt == 0), stop=(kt == 7),
                     r=[b_we[i], b_hx], w=[bpg], inc=(kt == 7))
            psu, bpu = k.bank()
            for kt in range(8):
                k.mm(psu[:, 0:NS], weu[i][:, kt, ht * 128:(ht + 1) * 128], hx[:, kt, :], start=(kt == 0), stop=(kt == 7),
                     r=[b_we[i], b_hx], w=[bpu], inc=(kt == 7))
            k.actf(sgt[j][:], psg[:, 0:NS], AF.Silu, r=[bpg], w=[b_sgt[j]])
            k.tt("dve", hid[:, ht, :], psu[:, 0:NS], sgt[j][:], ALU.mult, r=[bpu, b_sgt[j]], w=[b_hid])
        for blk in range(NBLK):
            seq = blk // (CAP // 128)
            r0 = e * CAP + (blk % (CAP // 128)) * 128
            j = bi % 2
            bi += 1
            for half in range(2):
                ps, bp = k.bank()
                for kt in range(4):
                    k.mm(ps[:, :], hid[:, kt, blk * 128:(blk + 1) * 128], wed[i][:, kt, half * 512:(half + 1) * 512],
                         start=(kt == 0), stop=(kt == 3), r=[b_hid, b_we[i]], w=[bp], inc=(kt == 3))
                hs = slice(half * 512, (half + 1) * 512)
                if half == 0:
                    k.cp("act", yt[j][:, hs], ps[:, :], r=[bp], w=[b_yt[j]])
                else:
                    k.cp("dve", yt[j][:, hs], ps[:, :], r=[bp], w=[b_yt[j]])
            by = Buf()
            b_ys.append(by)
            k.dma(ybuf_d[seq][r0:r0 + 128, :], yt[j][:], r=[b_yt[j]], w=[by])
    k.release(mX)

    xo = [k.sb(f"xo{i}", [128, 1024]) for i in range(2)]
    b_xo = [Buf(), Buf()]
    yg = [[k.sb(f"yg{i}_{c}", [128, 1024]) for c in range(2)] for i in range(2)]
    b_yg = [[Buf(), Buf()] for i in range(2)]
    for t in range(NT):
        i = t % 2
        seq = t // NTT
        r0 = t * 128
        k.dma(xo[i][:], x1_d[r0:r0 + 128, :], r=[L["x1_bufs"][t]], w=[b_xo[i]])
        for c in range(2):
            k.memset("pool", yg[i][c][:], 0.0, w=[b_yg[i][c]])
            off = bass.IndirectOffsetOnAxis(ap=desti[:, t, c:c + 1], axis=0)
            ydst = yg[i][c]
            ysrc = ybuf_d[seq]
            k.fw.dma(k.fw.pool, (lambda off=off, ydst=ydst, ysrc=ysrc: nc.gpsimd.indirect_dma_start(
                out=ydst[:, :], out_offset=None, in_=ysrc[:, :], in_offset=off,
                bounds_check=bc_reg(), oob_is_err=False)), reads=b_ys + [b_route[t]], writes=[b_yg[i][c]])
        k.ts("dve", yg[i][0][:], yg[i][0][:], wts[:, t, 0:1], None, ALU.mult, r=[b_yg[i][0], b_route[t]], w=[b_yg[i][0]])
        k.stt("dve", yg[i][0][:], yg[i][1][:], wts[:, t, 1:2], yg[i][0][:], ALU.mult, ALU.add,
              r=[b_yg[i][0], b_yg[i][1], b_route[t]], w=[b_yg[i][0]])
        k.tt("pool", yg[i][0][:], yg[i][0][:], g2r[:, seq, :], ALU.mult, r=[b_yg[i][0], b_g2], w=[b_yg[i][0]])
        k.tt("dve", xo[i][:], xo[i][:], yg[i][0][:], ALU.add, r=[b_yg[i][0], b_xo[i]], w=[b_xo[i]])
        finals.append(k.dma(out_d[r0:r0 + 128, :], xo[i][:], r=[b_xo[i]], w=[Buf()]))


def _consts(NSEQ=2):
    c = np.zeros((128, 1280), np.float32)
    tp = np.arange(128)[:, None]
    tf = np.arange(128)[None, :]
    c[:, 1088:1216] = (tp <= tf)
    for b in range(NSEQ):
        c[:, 1216 + b * 32:1216 + (b + 1) * 32] = np.arange(32)[None, :] * 256 - 1.0
    c[:, 0:128] = np.eye(128)
    bo = np.zeros((128, 128), np.float32)
    bo[:64, :64] = 1
    bo[64:, 64:] = 1
    c[:, 128:256] = bo
    j = np.arange(128)[:, None]
    s = np.arange(128)[None, :]
    same = (j // 64) == (s // 64)
    strict = same & ((j % 64) < (s % 64))
    incl = same & ((j % 64) <= (s % 64))
    c[:, 256:384] = strict
    c[:, 384:512] = incl
    c[:, 512:640] = strict
    c[:, 640:768] = incl
    c[:, 768:896] = same & ((s % 64) < (j % 64))
    ii = np.zeros((128, 64), np.float32)
    ii[np.arange(128), np.arange(128) % 64] = 1
    c[:, 896:960] = ii
    c[:, 960:1088] = 1.0
    return c


def _attn_tables(rel_bias):
    kl = np.arange(128)[:, None, None]
    j = np.arange(5)[None, :, None]
    q = np.arange(128)[None, None, :]
    off = (j - 4) * 128 + kl - q
    idx = np.clip(off, -128, 128) + 128
    tab = rel_bias[:, idx]
    tab = np.ascontiguousarray(tab.transpose(1, 0, 2, 3)).reshape(128, 8, 640)
    kc = 2 * j + kl // 64
    qc = q // 64
    vis = (kc >= qc) & (kc <= qc + 8)
    mask = np.where(vis, 0.0, -30000.0).astype(np.float32).reshape(128, 640)
    return tab.astype(np.float32), mask


def _col(v, n):
    return np.ascontiguousarray(np.asarray(v, np.float32).reshape(n, 128).T)


def prep_inputs(inputs, NSEQ=2, S=2048, ncores=NCORES):
    f = lambda n: np.asarray(inputs[n], np.float32)
    shared = {}
    shared["w_ada"] = np.ascontiguousarray(f("w_ada")[0])
    shared["b_ada_col"] = _col(f("b_ada")[0], 48)
    shared["b_ada_row"] = np.ascontiguousarray(f("b_ada")[0].reshape(1, -1))
    shared["ncols"] = np.concatenate([_col(f("norm1_g")[0], 8), _col(f("norm2_g")[0], 8)], axis=1)
    shared["w_in"] = np.ascontiguousarray(f("w_in")[0])
    mu = f("rwkv_mu")[0]
    mc = np.zeros((128, 15), np.float32)
    mc[:, 0:12] = _col(mu[0:1536], 12)
    mc[0:64, 12] = mu[1536:1600]
    mc[0:64, 13] = mu[1600:1664]
    mc[:, 14] = mu[1664:1792]
    shared["mu_cols"] = mc
    rw = [f("rwkv_w0")[0], f("rwkv_a0")[0], f("rwkv_k_k")[0], f("rwkv_k_a")[0], f("rwkv_r_k")[0].reshape(-1),
          f("rwkv_lnx_g")[0], f("rwkv_lnx_b")[0]]
    shared["rw_cols"] = np.concatenate([_col(v, 4) for v in rw], axis=1)
    shared["w_up"] = np.ascontiguousarray(f("rwkv_w_up")[0])
    shared["a_up"] = np.ascontiguousarray(f("rwkv_a_up")[0])
    shared["g_up"] = np.ascontiguousarray(f("rwkv_g_up")[0])
    shared["qkg"] = np.stack([np.tile(f("attn_q_g")[0], 2), np.tile(f("attn_k_g")[0], 2)], axis=1).astype(np.float32)
    tab, mask = _attn_tables(f("attn_rel_bias")[0])
    shared["btab"] = tab
    shared["bmask"] = mask
    shared["w_br"] = np.ascontiguousarray(f("w_branch_rwkv")[0])
    shared["w_ba"] = np.ascontiguousarray(f("w_branch_attn")[0])
    shared["w_out"] = np.ascontiguousarray(f("w_out")[0])
    shared["w_router"] = np.ascontiguousarray(np.concatenate([f("router_coarse_w")[0], f("router_fine_w")[0]], axis=1))
    shared["b_router"] = np.concatenate([f("router_coarse_b")[0], f("router_fine_b")[0]]).reshape(1, 36).astype(np.float32)
    shared["e_gate"] = np.ascontiguousarray(f("expert_w_gate")[0])
    shared["e_up"] = np.ascontiguousarray(f("expert_w_up")[0])
    shared["e_down"] = np.ascontiguousarray(f("expert_w_down")[0])
    shared["consts"] = _consts(NSEQ)
    x = f("x")
    c = f("c")
    maps = []
    for i in range(ncores):
        m = dict(shared)
        m["x"] = np.ascontiguousarray(x[i * NSEQ:(i + 1) * NSEQ, :S].reshape(NSEQ * S, D))
        cT = c[i * NSEQ:(i + 1) * NSEQ].T
        m["cT"] = np.ascontiguousarray(cT.reshape(8, 128, NSEQ).transpose(1, 0, 2))
        maps.append(m)
    return maps


_NC_CACHE = {}


def kernel(**inputs):
    NSEQ, S = 2, 2048
    if "nc" not in _NC_CACHE:
        _NC_CACHE["nc"] = build(NSEQ, S)
    nc = _NC_CACHE["nc"]
    maps = prep_inputs(inputs, NSEQ, S)
    res = run_bass_kernel_spmd(nc, maps, core_ids=list(range(NCORES)))
    out = np.concatenate([np.asarray(r["out"], np.float32).reshape(NSEQ, S, D) for r in res.results], axis=0)
    return out
```
